# Optimizing a Trainium2 kernel written in Bass

```python
import jax, jax.numpy as jnp
from jax import lax
import numpy as np

D_MODEL = 2048
BATCH = 4
SEQ = 2048
DEPTH = 4

GRID_W = 64
CTX_LEN = 256
N_MIXERS = 2
N_HEADS = 16
HEAD_DIM = D_MODEL // N_HEADS
NA_MAX_ROWS = 8
NA_COLS = 16
CONV_WIDTH = 3
N_EXPERTS = 16
EC_FACTOR = 2
D_FF_EXPERT = D_MODEL // 2
N_MOD = 6
RMS_EPS = 1e-6
NEG_INF = -1e30
N_CONV_LAYERS = (DEPTH + N_MIXERS - 1) // N_MIXERS
N_NA_LAYERS = DEPTH // N_MIXERS

kernel_name = "hybrid_shortconv_natten_ecmoe_dit"


def rmsnorm(x, g):
    xf = x.astype(jnp.float32)
    y = xf * lax.rsqrt(jnp.mean(xf * xf, axis=-1, keepdims=True) + RMS_EPS)
    return (y * g.astype(jnp.float32)).astype(x.dtype)


def modulate(h, shift, scale):
    return h * (1 + scale[:, None]) + shift[:, None]


def depthwise_conv(u, w):
    pad = CONV_WIDTH // 2
    return lax.conv_general_dilated(u, w[:, None, :], (1,), [(pad, pad)],
                                    dimension_numbers=('NWC', 'WIO', 'NWC'),
                                    feature_group_count=u.shape[-1])


def short_conv_mixer(h, w_in, w_conv, w_out):
    b_gate, c_gate, u = jnp.split(h @ w_in, 3, axis=-1)
    return (b_gate * depthwise_conv(c_gate * u, w_conv)) @ w_out


def project_heads(h, w):
    B, N, _ = h.shape
    n = w.shape[-1] // D_MODEL
    return (h @ w).reshape(B, N, n, N_HEADS, HEAD_DIM)


def context_self_attention(q, k, v):
    s = jnp.einsum('bqhd,bkhd->bhqk', q, k).astype(jnp.float32) * (HEAD_DIM ** -0.5)
    p = jax.nn.softmax(s, axis=-1).astype(v.dtype)
    return jnp.einsum('bhqk,bkhd->bqhd', p, v)


def neighbourhood_attention(q, k, v, k_ctx, v_ctx, rpb):
    B, S, H, Dh = q.shape
    rows = S // GRID_W
    kh = min(NA_MAX_ROWS, rows)
    kw = NA_COLS
    n_loc = kh * GRID_W
    scale = Dh ** -0.5
    q_rows = q.reshape(B, rows, GRID_W, H, Dh).transpose(1, 0, 2, 3, 4)
    kg = k.reshape(B, rows, GRID_W, H, Dh)
    vg = v.reshape(B, rows, GRID_W, H, Dh)
    col = jnp.arange(GRID_W)
    col_start = jnp.clip(col - kw // 2, 0, GRID_W - kw)
    col_in = (col[None, :] >= col_start[:, None]) & (col[None, :] < col_start[:, None] + kw)
    col_mask = jnp.broadcast_to(col_in[:, None, :], (GRID_W, kh, GRID_W)).reshape(GRID_W, n_loc)
    dc_idx = jnp.clip(col[None, :] - col[:, None] + NA_COLS - 1, 0, 2 * NA_COLS - 2)
    row_ids = jnp.arange(rows)
    row_start = jnp.clip(row_ids - kh // 2, 0, rows - kh)

    def one_row(args):
        q_r, r, s0 = args
        k_blk = lax.dynamic_slice_in_dim(kg, s0, kh, axis=1).reshape(B, n_loc, H, Dh)
        v_blk = lax.dynamic_slice_in_dim(vg, s0, kh, axis=1).reshape(B, n_loc, H, Dh)
        dr_idx = s0 + jnp.arange(kh) - r + NA_MAX_ROWS - 1
        bias = rpb[:, dr_idx][:, :, dc_idx]
        bias = bias.transpose(0, 2, 1, 3).reshape(H, GRID_W, n_loc).astype(jnp.float32)
        s_loc = jnp.einsum('bqhd,bkhd->bhqk', q_r, k_blk).astype(jnp.float32) * scale + bias
        s_loc = jnp.where(col_mask, s_loc, NEG_INF)
        s_ctx = jnp.einsum('bqhd,bkhd->bhqk', q_r, k_ctx).astype(jnp.float32) * scale
        p = jax.nn.softmax(jnp.concatenate([s_loc, s_ctx], axis=-1), axis=-1).astype(v.dtype)
        return (jnp.einsum('bhqk,bkhd->bqhd', p[..., :n_loc], v_blk)
                + jnp.einsum('bhqk,bkhd->bqhd', p[..., n_loc:], v_ctx))

    out = lax.map(one_row, (q_rows, row_ids, row_start))
    return out.transpose(1, 0, 2, 3, 4).reshape(B, S, H * Dh)


def expert_choice_moe(h, w_router, w1, w3, w2):
    B, N, D = h.shape
    cap = max(1, EC_FACTOR * N // N_EXPERTS)
    aff = jax.nn.softmax((h @ w_router).astype(jnp.float32), axis=-1)
    g, idx = lax.top_k(aff.transpose(0, 2, 1), cap)
    xe = jax.vmap(lambda hb, ib: hb[ib])(h, idx)
    a = jnp.einsum('becd,edf->becf', xe, w1)
    u = jnp.einsum('becd,edf->becf', xe, w3)
    y = jnp.einsum('becf,efd->becd', jax.nn.silu(a) * u, w2) * g[..., None].astype(h.dtype)
    bidx = jnp.arange(B)[:, None, None]
    return jnp.zeros_like(h).at[bidx, idx].add(y)


def setup_inputs(seed: int = 0) -> dict:
    key = jax.random.key(seed)
    ks = jax.random.split(key, 20)
    D, F, E = D_MODEL, D_FF_EXPERT, N_EXPERTS
    nrm = jax.random.normal
    return {
        'x': nrm(ks[0], (BATCH, SEQ, D), jnp.float32),
        'c': nrm(ks[1], (BATCH, D), jnp.float32),
        'ctx': nrm(ks[2], (BATCH, CTX_LEN, D), jnp.float32),
        'c_ctx': nrm(ks[3], (D,), jnp.float32),
        'w_ada': nrm(ks[4], (DEPTH, D, N_MOD * D), jnp.float32) * (0.5 * D ** -0.5),
        'b_ada': nrm(ks[5], (DEPTH, N_MOD * D), jnp.float32) * 0.01,
        'norm_g': 1.0 + 0.02 * nrm(ks[6], (DEPTH, 2, D), jnp.float32),
        'conv_w_in': nrm(ks[7], (N_CONV_LAYERS, D, 3 * D), jnp.float32) * D ** -0.5,
        'conv_w': nrm(ks[8], (N_CONV_LAYERS, CONV_WIDTH, D), jnp.float32) * CONV_WIDTH ** -0.5,
        'conv_w_out': nrm(ks[9], (N_CONV_LAYERS, D, D), jnp.float32) * D ** -0.5,
        'attn_w_qkv': nrm(ks[10], (N_NA_LAYERS, D, 3 * D), jnp.float32) * D ** -0.5,
        'attn_w_out': nrm(ks[11], (N_NA_LAYERS, D, D), jnp.float32) * D ** -0.5,
        'attn_rpb': 0.1 * nrm(ks[12], (N_NA_LAYERS, N_HEADS, 2 * NA_MAX_ROWS - 1, 2 * NA_COLS - 1), jnp.float32),
        'w_router': nrm(ks[13], (DEPTH, D, E), jnp.float32) * D ** -0.5,
        'w1': nrm(ks[14], (DEPTH, E, D, F), jnp.float32) * D ** -0.5,
        'w3': nrm(ks[15], (DEPTH, E, D, F), jnp.float32) * D ** -0.5,
        'w2': nrm(ks[16], (DEPTH, E, F, D), jnp.float32) * F ** -0.5,
        'final_g': 1.0 + 0.02 * nrm(ks[17], (D,), jnp.float32),
    }


def reference(x, c, ctx, c_ctx, w_ada, b_ada, norm_g, conv_w_in, conv_w, conv_w_out,
              attn_w_qkv, attn_w_out, attn_rpb, w_router, w1, w3, w2, final_g):
    B, S, D = x.shape
    Nc = ctx.shape[1]
    silu_c = jax.nn.silu(c)
    silu_cc = jax.nn.silu(c_ctx)[None]
    for i in range(DEPTH):
        last = i == DEPTH - 1
        j = i // N_MIXERS
        sh1, sc1, g1, sh2, sc2, g2 = jnp.split(silu_c @ w_ada[i] + b_ada[i], N_MOD, axis=-1)
        csh1, csc1, cg1, csh2, csc2, cg2 = jnp.split(silu_cc @ w_ada[i] + b_ada[i], N_MOD, axis=-1)
        hx = modulate(rmsnorm(x, norm_g[i, 0]), sh1, sc1)
        hc = modulate(rmsnorm(ctx, norm_g[i, 0]), csh1, csc1)
        if i % N_MIXERS == 0:
            yx = short_conv_mixer(hx, conv_w_in[j], conv_w[j], conv_w_out[j])
            yc = None if last else short_conv_mixer(hc, conv_w_in[j], conv_w[j], conv_w_out[j])
        else:
            qkv_x = project_heads(hx, attn_w_qkv[j])
            if last:
                kv_c = project_heads(hc, attn_w_qkv[j][:, D_MODEL:])
                k_c, v_c = kv_c[:, :, 0], kv_c[:, :, 1]
                yc = None
            else:
                qkv_c = project_heads(hc, attn_w_qkv[j])
                q_c, k_c, v_c = qkv_c[:, :, 0], qkv_c[:, :, 1], qkv_c[:, :, 2]
                yc = context_self_attention(q_c, k_c, v_c).reshape(B, Nc, D) @ attn_w_out[j]
            yx = neighbourhood_attention(qkv_x[:, :, 0], qkv_x[:, :, 1], qkv_x[:, :, 2],
                                         k_c, v_c, attn_rpb[j]) @ attn_w_out[j]
        x = x + g1[:, None] * yx
        x = x + g2[:, None] * expert_choice_moe(modulate(rmsnorm(x, norm_g[i, 1]), sh2, sc2),
                                               w_router[i], w1[i], w3[i], w2[i])
        if not last:
            ctx = ctx + cg1[:, None] * yc
            ctx = ctx + cg2[:, None] * expert_choice_moe(modulate(rmsnorm(ctx, norm_g[i, 1]), csh2, csc2),
                                                         w_router[i], w1[i], w3[i], w2[i])
    return rmsnorm(x, final_g)
```

```python
import contextlib
import numpy as np
import concourse.bass as bass
import concourse.mybir as mybir
from concourse.bass_utils import run_bass_kernel_spmd

F32 = mybir.dt.float32
BF16 = mybir.dt.bfloat16
U32 = mybir.dt.uint32
I32 = mybir.dt.int32
AF = mybir.ActivationFunctionType
ALU = mybir.AluOpType
AX = mybir.AxisListType

D = 2048
TL = 2048
TC = 256
T = TL + TC
KC = 16
NE = 16
FF = 1024
DEPTH = 4
CAP_L = 256
CAP_C = 32
NS = CAP_L + CAP_C
EPS = 1e-6
TGS = [(0, 512), (512, 512), (1024, 512), (1536, 512), (2048, 256)]
BLKS = [(t0, 256) for t0 in range(0, T, 256)]
SCALE = 128 ** -0.5

COMPUTE = ("pe", "act", "dve", "pool")
ALLQ = ("pe", "act", "dve", "pool", "sp")


def var(t0):
    return 0 if t0 < TL else 1


class Prog:
    def __init__(self, nc):
        self.nc = nc
        self.eng = {"pe": nc.tensor, "act": nc.scalar, "dve": nc.vector, "pool": nc.gpsimd, "sp": nc.sync}
        self.csem = {e: nc.alloc_semaphore(name=f"cs_{e}") for e in COMPUTE}
        self.ccnt = {e: 0 for e in COMPUTE}
        self.dsem = {}
        self.dcnt = {}
        self.waited = {e: {} for e in ALLQ}
        self.res = {}
        self.sems = {f"cs_{e}": self.csem[e] for e in COMPUTE}
        self.n_instr = 0
        self.n_wait = 0
        self.dma_rr = 0

    def _deps(self, reads, writes):
        deps = []
        for r in reads:
            st = self.res.get(r)
            if st and st[0] is not None:
                deps.append(st[0])
        for w in writes:
            st = self.res.get(w)
            if st:
                if st[0] is not None:
                    deps.append(st[0])
                deps.extend(st[1])
        return deps

    def _emit_waits(self, e, deps, skip_sem=None):
        need = {}
        for (sn, v) in deps:
            if sn == skip_sem:
                continue
            if self.waited[e].get(sn, 0) >= v:
                continue
            if need.get(sn, 0) < v:
                need[sn] = v
        for sn, v in need.items():
            self.waited[e][sn] = v
            self.eng[e].wait_ge(self.sems[sn], v)
            self.n_wait += 1

    def _record(self, dep, reads, writes):
        for r in reads:
            st = self.res.setdefault(r, [None, []])
            st[1].append(dep)
        for w in writes:
            self.res[w] = [dep, []]

    def op(self, e, fn, reads=(), writes=()):
        deps = self._deps(reads, writes)
        skip = "cs_pe" if e == "pe" else None
        self._emit_waits(e, deps, skip_sem=skip)
        self.ccnt[e] += 1
        fn(self.eng[e]).then_inc(self.csem[e], 1)
        self._record((f"cs_{e}", self.ccnt[e]), reads, writes)
        self.n_instr += 1

    def dma(self, qe, fn, reads=(), writes=(), sem_key=None):
        key = sem_key if sem_key is not None else writes[0]
        if key not in self.dsem:
            name = f"ds_{len(self.dsem)}"
            h = self.nc.alloc_semaphore(name=name)
            self.dsem[key] = (h, name)
            self.sems[name] = h
            self.dcnt[name] = 0
        h, name = self.dsem[key]
        deps = self._deps(reads, writes)
        self._emit_waits(qe, deps)
        self.dcnt[name] += 16
        fn(self.eng[qe]).then_inc(h, 16)
        self._record((name, self.dcnt[name]), reads, writes)
        self.n_instr += 1

    def barrier(self):
        targets = [(f"cs_{e}", self.ccnt[e]) for e in COMPUTE] + list(self.dcnt.items())
        for e in ALLQ:
            self._emit_waits(e, targets)
        self.res.clear()


class Ctx:
    pass


_UID = [0]


def uname(name):
    _UID[0] += 1
    return f"{name}_u{_UID[0]}"


def build_program(n_layers=DEPTH, debug=False, moe=True, final=True, ada=True, x0=True, layers=None):
    nc = bass.Bass("TRN2", target_bir_lowering=False)
    g = Ctx()
    g.nc = nc
    ext = lambda name, shape, dt=F32: nc.dram_tensor(name, shape, dt, kind="ExternalInput").ap()
    g.x_in = ext("x_in", [TL, D])
    g.ctx_in = ext("ctx_in", [TC, D])
    g.cT = ext("cT", [128, KC, 2])
    g.badaT = ext("badaT", [128, DEPTH, 96])
    g.normgT = ext("normgT", [128, DEPTH, 2, KC])
    g.finalgT = ext("finalgT", [128, KC])
    g.convwT = ext("convwT", [128, 2, 3, KC])
    layers = list(range(n_layers)) if layers is None else list(layers)
    g.layers = layers
    g.names = []
    g.w_ada, g.w_router, g.w1, g.w3, g.w2 = {}, {}, {}, {}, {}
    g.conv_w_in, g.conv_w_out, g.attn_w_qkv, g.attn_w_out, g.biasT = {}, {}, {}, {}, {}
    for l in layers:
        j = l // 2
        g.w_ada[l] = ext(f"w_ada{l}", [D, 6 * D]); g.names.append(f"w_ada{l}")
        if l % 2 == 0:
            g.conv_w_in[j] = ext(f"conv_w_in{j}", [D, 3 * D]); g.conv_w_out[j] = ext(f"conv_w_out{j}", [D, D])
            g.names += [f"conv_w_in{j}", f"conv_w_out{j}"]
        else:
            g.attn_w_qkv[j] = ext(f"attn_w_qkv{j}", [D, 3 * D]); g.attn_w_out[j] = ext(f"attn_w_out{j}", [D, D])
            g.biasT[j] = ext(f"biasT{j}", [NE, 64, 960])
            g.names += [f"attn_w_qkv{j}", f"attn_w_out{j}", f"biasT{j}"]
        if moe:
            g.w_router[l] = ext(f"w_router{l}", [D, NE])
            g.w1[l] = ext(f"w1_{l}", [NE, D, FF]); g.w3[l] = ext(f"w3_{l}", [NE, D, FF]); g.w2[l] = ext(f"w2_{l}", [NE, FF, D])
            g.names += [f"w_router{l}", f"w1_{l}", f"w3_{l}", f"w2_{l}"]
    g.out = nc.dram_tensor("out", [TL, D], F32, kind="ExternalOutput").ap()
    skind = "Internal"
    g.xT = nc.dram_tensor("xT", [D, T], F32, kind=skind).ap()
    g.zT = nc.dram_tensor("zT", [D, T], BF16, kind=skind).ap()
    g.ybuf = nc.dram_tensor("ybuf", [NE, 3, 128, D], BF16, kind=skind).ap()
    g.scr_idx = nc.dram_tensor("scr_idx", [NE * NS], F32, kind=skind).ap()
    g.dbg = {}

    with nc.cleanup_on_exit():
        P = Prog(nc)
        g.P = P
        with contextlib.ExitStack() as es:
            sb = lambda name, shape, dt: es.enter_context(nc.sbuf_tensor(uname(name), shape, dt))
            g.A = sb("Abuf", [128, KC * T], BF16)
            g.identF = sb("identF", [128, 128], F32)
            g.identB = sb("identB", [128, 128], BF16)
            g.onesF = sb("onesF", [128, 128], F32)
            g.mod = sb("mod", [128, DEPTH, 96, 2], F32)
            g.cA = sb("cA", [128, DEPTH, 2, KC, 2], F32)
            g.cB = sb("cB", [128, DEPTH, 2, KC, 2], F32)
            g.cG = sb("cG", [128, DEPTH, 2, KC, 2], F32)
            g.normg = sb("normg", [128, DEPTH, 2, KC], F32)
            g.finalg = sb("finalg", [128, KC], F32)
            g.convw = sb("convw", [128, 2, 3, KC], F32)
            g.iotaP = sb("iotaP", [128, KC], F32)
            g.epsT = sb("epsT", [128, 1], F32)
            g.affT = sb("affT", [NE, T], F32)
            g.idx_bc = sb("idx_bc", [128, NE * NS], F32)
            g.idx_sl = sb("idx_sl", [128, NE, 3], F32)
            g.g_sl = sb("g_sl", [128, NE, 3], F32)
            stage_consts(g)
            if ada:
                stage_ada(g)
            if x0:
                stage_x0(g)
            for l in g.layers:
                j = l // 2
                stage_norm(g, l, 0)
                if l % 2 == 0:
                    stage_conv(g, l, j)
                    stage_outproj(g, l, g.conv_w_out[j])
                else:
                    stage_attn(g, l, j)
                    stage_outproj(g, l, g.attn_w_out[j])
                if debug:
                    snapshot(g, 2 * l)
                if moe:
                    stage_norm(g, l, 1)
                    stage_topk(g, l)
                    stage_ffn(g, l)
                    stage_scatter(g, l)
                    if debug:
                        snapshot(g, 2 * l + 1)
            if final:
                stage_final(g)
            P.barrier()
    g.stats = (P.n_instr, P.n_wait, len(P.dsem))
    return nc, g


def snapshot(g, k):
    P = g.P
    g.dbg[k] = g.nc.dram_tensor(f"dbg{k}", [D, T], F32, kind="ExternalOutput").ap()
    for q in range(4):
        P.dma("sp", lambda e, q=q: e.dma_start(out=g.dbg[k][q * 512:(q + 1) * 512, :], in_=g.xT[q * 512:(q + 1) * 512, :]), reads=["xT"], writes=[f"dbg{k}"])
    P.barrier()


def stage_consts(g):
    nc, P = g.nc, g.P
    with contextlib.ExitStack() as es:
        ti = es.enter_context(nc.sbuf_tensor(uname("c_ti"), [128, TL], I32))
        P.op("pool", lambda e: e.iota(ti[:, 0:128], pattern=[[1, 128]], base=0, channel_multiplier=-1), writes=["c_ti"])
        P.op("dve", lambda e: e.tensor_single_scalar(out=g.identF[:], in_=ti[:, 0:128], scalar=0, op=ALU.is_equal),
             reads=["c_ti"], writes=["identF"])
        P.op("dve", lambda e: e.tensor_copy(out=g.identB[:], in_=g.identF[:]), reads=["identF"], writes=["identB"])
        P.op("dve", lambda e: e.memset(g.onesF[:], 1.0), writes=["onesF"])
        P.op("dve", lambda e: e.memset(g.epsT[:], EPS), writes=["epsT"])
        P.op("pool", lambda e: e.iota(ti[:, 0:KC], pattern=[[128, KC]], base=0, channel_multiplier=1), reads=["identF"], writes=["c_ti"])
        P.op("dve", lambda e: e.tensor_copy(out=g.iotaP[:], in_=ti[:, 0:KC]), reads=["c_ti"], writes=["iotaP"])
        P.dma("sp", lambda e: e.dma_start(out=g.normg[:], in_=g.normgT), writes=["normg"])
        P.dma("sp", lambda e: e.dma_start(out=g.finalg[:], in_=g.finalgT), writes=["finalg"])
        P.dma("sp", lambda e: e.dma_start(out=g.convw[:], in_=g.convwT), writes=["convw"])
        P.barrier()


def stage_ada(g):
    nc, P = g.nc, g.P
    with contextlib.ExitStack() as es:
        sb = lambda name, shape, dt: es.enter_context(nc.sbuf_tensor(uname(name), shape, dt))
        cT = sb("a_cT", [128, KC, 2], F32)
        sig = sb("a_sig", [128, KC, 2], F32)
        csT = sb("a_csT", [128, KC, 2], BF16)
        bada = sb("a_bada", [128, DEPTH, 96], F32)
        wt = [sb(f"a_wt{i}", [128, KC, 512], BF16) for i in range(2)]
        ps = es.enter_context(nc.psum_tensor(uname("a_ps"), [128, 96, 2], F32))
        P.dma("sp", lambda e: e.dma_start(out=cT[:], in_=g.cT), writes=["a_cT"])
        P.dma("sp", lambda e: e.dma_start(out=bada[:], in_=g.badaT), writes=["a_bada"])
        P.op("act", lambda e: e.activation(out=sig[:], in_=cT[:], func=AF.Sigmoid), reads=["a_cT"], writes=["a_sig"])
        P.op("dve", lambda e: e.tensor_tensor(out=csT[:], in0=cT[:], in1=sig[:], op=ALU.mult), reads=["a_cT", "a_sig"], writes=["a_csT"])
        n = 0
        for l in g.layers:
            for pc in range(24):
                w = wt[n % 2]
                wk = f"a_wt{n % 2}"
                src = g.w_ada[l].rearrange("(kc p) n -> p kc n", p=128)[:, :, pc * 512:(pc + 1) * 512]
                P.dma("pool", lambda e, w=w, src=src: e.dma_start(out=w[:], in_=src), writes=[wk])
                for q in range(4):
                    vc = pc * 4 + q
                    for kc in range(KC):
                        P.op("pe", lambda e, w=w, q=q, kc=kc, vc=vc, l=l: e.matmul(
                            ps[:, vc, :], lhsT=w[:, kc, q * 128:(q + 1) * 128], rhs=csT[:, kc, :],
                            start=(kc == 0), stop=(kc == KC - 1)), reads=[wk, "a_csT"], writes=["a_ps"])
                n += 1
            for v in range(2):
                P.op("dve", lambda e, l=l, v=v: e.tensor_tensor(out=g.mod[:, l, :, v], in0=ps[:, :, v], in1=bada[:, l, :], op=ALU.add),
                     reads=["a_ps", "a_bada"], writes=["mod"])
            for s in range(2):
                for v in range(2):
                    sh = g.mod[:, l, (3 * s) * KC:(3 * s + 1) * KC, v]
                    sc = g.mod[:, l, (3 * s + 1) * KC:(3 * s + 2) * KC, v]
                    gt = g.mod[:, l, (3 * s + 2) * KC:(3 * s + 3) * KC, v]
                    P.op("dve", lambda e, l=l, s=s, v=v, sc=sc: e.scalar_tensor_tensor(
                        out=g.cA[:, l, s, :, v], in0=sc, scalar=1.0, in1=g.normg[:, l, s, :], op0=ALU.add, op1=ALU.mult),
                        reads=["mod", "normg"], writes=["cA"])
                    P.op("dve", lambda e, l=l, s=s, v=v, sh=sh: e.tensor_copy(out=g.cB[:, l, s, :, v], in_=sh), reads=["mod"], writes=["cB"])
                    P.op("dve", lambda e, l=l, s=s, v=v, gt=gt: e.tensor_copy(out=g.cG[:, l, s, :, v], in_=gt), reads=["mod"], writes=["cG"])
        P.barrier()


def stage_x0(g):
    nc, P = g.nc, g.P
    with contextlib.ExitStack() as es:
        sb = lambda name, shape, dt: es.enter_context(nc.sbuf_tensor(uname(name), shape, dt))
        xin = [sb(f"x0_in{i}", [128, D], F32) for i in range(2)]
        xo = [sb(f"x0_o{i}", [128, KC, 128], F32) for i in range(2)]
        ps = [es.enter_context(nc.psum_tensor(uname(f"x0_ps{i}"), [128, 4, 128], F32)) for i in range(4)]
        xTv = g.xT.rearrange("(kc p) t -> p kc t", p=128)
        pi = 0
        for tc in range(T // 128):
            i = tc % 2
            src = g.x_in[tc * 128:(tc + 1) * 128, :] if tc < 16 else g.ctx_in[(tc - 16) * 128:(tc - 15) * 128, :]
            P.dma("sp", lambda e, i=i, src=src: e.dma_start(out=xin[i][:], in_=src), writes=[f"x0_in{i}"])
            for q4 in range(4):
                pp = ps[pi % 4]
                pk = f"x0_ps{pi % 4}"
                pi += 1
                for q in range(4):
                    dc = q4 * 4 + q
                    P.op("pe", lambda e, pp=pp, q=q, dc=dc, i=i: e.transpose(out=pp[:, q, :], in_=xin[i][:, dc * 128:(dc + 1) * 128], identity=g.identF[:]),
                         reads=[f"x0_in{i}", "identF"], writes=[pk])
                eng = "dve" if q4 % 2 == 0 else "act"
                if eng == "dve":
                    P.op("dve", lambda e, pp=pp, q4=q4, i=i: e.tensor_copy(out=xo[i][:, q4 * 4:(q4 + 1) * 4, :], in_=pp[:]), reads=[pk], writes=[f"x0_o{i}_{q4}"])
                else:
                    P.op("act", lambda e, pp=pp, q4=q4, i=i: e.copy(out=xo[i][:, q4 * 4:(q4 + 1) * 4, :], in_=pp[:]), reads=[pk], writes=[f"x0_o{i}_{q4}"])
            P.dma("sp", lambda e, i=i, tc=tc: e.dma_start(out=xTv[:, :, tc * 128:(tc + 1) * 128], in_=xo[i][:]),
                  reads=[f"x0_o{i}_{q}" for q in range(4)], writes=["xT"])
        P.barrier()


def norm_block(g, es_tiles, l, s, t0, W, i, want_f32=None):
    nc, P = g.nc, g.P
    xb, sq, rstd, tmp, ps = es_tiles
    v = var(t0)
    xTv = g.xT.rearrange("(kc p) t -> p kc t", p=128)
    P.dma("sp", lambda e: e.dma_start(out=xb[i][:, :, 0:W], in_=xTv[:, :, t0:t0 + W]), reads=["xT"], writes=[f"n_xb{i}"])
    P.op("act", lambda e: e.activation(out=sq[i][:, :, 0:W], in_=xb[i][:, :, 0:W], func=AF.Square), reads=[f"n_xb{i}"], writes=[f"n_sq{i}"])
    for kc in range(KC):
        P.op("pe", lambda e, kc=kc: e.matmul(ps[i][:, 0:W], lhsT=g.onesF[:], rhs=sq[i][:, kc, 0:W], start=(kc == 0), stop=(kc == KC - 1)),
             reads=[f"n_sq{i}", "onesF"], writes=[f"n_ps{i}"])
    P.op("act", lambda e: e.activation(out=rstd[i][:, 0:W], in_=ps[i][:, 0:W], func=AF.Sqrt, bias=g.epsT[:, 0:1], scale=1.0 / D),
         reads=[f"n_ps{i}", "epsT"], writes=[f"n_rstd{i}"])
    P.op("dve", lambda e: e.reciprocal(out=rstd[i][:, 0:W], in_=rstd[i][:, 0:W]), reads=[f"n_rstd{i}"], writes=[f"n_rstd{i}"])
    return v


def stage_norm(g, l, s):
    nc, P = g.nc, g.P
    with contextlib.ExitStack() as es:
        sb = lambda name, shape, dt: es.enter_context(nc.sbuf_tensor(uname(name), shape, dt))
        W = 256
        xb = [sb(f"n_xb{i}", [128, KC, W], F32) for i in range(2)]
        sq = [sb(f"n_sq{i}", [128, KC, W], F32) for i in range(2)]
        rstd = [sb(f"n_rstd{i}", [128, W], F32) for i in range(2)]
        ps = [es.enter_context(nc.psum_tensor(uname(f"n_ps{i}"), [128, 512], F32)) for i in range(2)]
        tiles = (xb, sq, rstd, None, ps)
        if s == 0:
            hT = g.A[:, :].rearrange("p (kc t) -> p kc t", kc=KC)
        else:
            htok = g.A[:, :].rearrange("p (tc d) -> p tc d", tc=T // 128)
            hb = [sb(f"n_hb{i}", [128, KC, W], BF16) for i in range(2)]
            wr = sb("n_wr", [128, KC, NE], F32)
            lg = [sb(f"n_lg{i}", [128, NE], F32) for i in range(2)]
            st = [sb(f"n_st{i}", [128, 4], F32) for i in range(2)]
            g_affT = g.affT
            psT = [es.enter_context(nc.psum_tensor(uname(f"n_psT{i}"), [128, 1024], BF16)) for i in range(2)]
            psR = [es.enter_context(nc.psum_tensor(uname(f"n_psR{i}"), [128, 512], F32)) for i in range(2)]
            P.dma("sp", lambda e: e.dma_start(out=wr[:], in_=g.w_router[l].rearrange("(kc p) n -> p kc n", p=128)), writes=["n_wr"])
        nT = 0
        for bi, (t0, _) in enumerate(BLKS):
            i = bi % 2
            v = norm_block(g, tiles, l, s, t0, W, i)
            for kc in range(KC):
                P.op("dve", lambda e, kc=kc: e.scalar_tensor_tensor(out=sq[i][:, kc, :], in0=xb[i][:, kc, :], scalar=g.cA[:, l, s, kc, v:v + 1],
                                                                   in1=rstd[i][:, :], op0=ALU.mult, op1=ALU.mult),
                     reads=[f"n_xb{i}", f"n_rstd{i}", "cA", f"n_ps{i}"], writes=[f"n_sq{i}"])
            if s == 0:
                for kc in range(KC):
                    P.op("act", lambda e, kc=kc: e.activation(out=hT[:, kc, t0:t0 + W], in_=sq[i][:, kc, :], func=AF.Identity,
                                                              bias=g.cB[:, l, s, kc, v:v + 1], scale=1.0),
                         reads=[f"n_sq{i}", "cB"], writes=["A"])
            else:
                for kc in range(KC):
                    P.op("act", lambda e, kc=kc: e.activation(out=sq[i][:, kc, :], in_=sq[i][:, kc, :], func=AF.Identity,
                                                              bias=g.cB[:, l, s, kc, v:v + 1], scale=1.0),
                         reads=[f"n_sq{i}", "cB"], writes=[f"n_sq{i}"])
                P.op("pool", lambda e: e.tensor_copy(out=hb[i][:], in_=sq[i][:]), reads=[f"n_sq{i}"], writes=[f"n_hb{i}"])
                for th in range(2):
                    tc = (t0 + th * 128) // 128
                    for kc in range(KC):
                        P.op("pe", lambda e, kc=kc, th=th: e.matmul(psR[i][:, th * 16:(th + 1) * 16], lhsT=sq[i][:, kc, th * 128:(th + 1) * 128], rhs=wr[:, kc, :],
                                                                    start=(kc == 0), stop=(kc == KC - 1)),
                             reads=[f"n_sq{i}", "n_wr"], writes=[f"n_psR{i}a"])
                    lgi = lg[i]
                    sti = st[i]
                    P.op("dve", lambda e, th=th: e.reduce_max(out=sti[:, 0:1], in_=psR[i][:, th * 16:(th + 1) * 16], axis=AX.X),
                         reads=[f"n_psR{i}a"], writes=[f"n_st{i}"])
                    P.op("dve", lambda e: e.tensor_scalar_mul(out=sti[:, 1:2], in0=sti[:, 0:1], scalar1=-1.0), reads=[f"n_st{i}"], writes=[f"n_st{i}"])
                    P.op("act", lambda e, th=th: e.activation(out=lgi[:], in_=psR[i][:, th * 16:(th + 1) * 16], func=AF.Exp, bias=sti[:, 1:2], scale=1.0,
                                                              accum_out=sti[:, 2:3]),
                         reads=[f"n_psR{i}a", f"n_st{i}"], writes=[f"n_lg{i}", f"n_st{i}"])
                    P.op("dve", lambda e: e.reciprocal(out=sti[:, 3:4], in_=sti[:, 2:3]), reads=[f"n_st{i}"], writes=[f"n_st{i}"])
                    P.op("dve", lambda e: e.tensor_scalar_mul(out=lgi[:], in0=lgi[:], scalar1=sti[:, 3:4]), reads=[f"n_st{i}", f"n_lg{i}"], writes=[f"n_lg{i}"])
                    P.op("pe", lambda e: e.transpose(out=psR[i][0:16, 128:256], in_=lgi[:], identity=g.identF[:]),
                         reads=[f"n_lg{i}", "identF"], writes=[f"n_psR{i}b"])
                    P.op("act", lambda e, tc=tc: e.copy(out=g_affT[:, tc * 128:(tc + 1) * 128], in_=psR[i][0:16, 128:256]),
                         reads=[f"n_psR{i}b"], writes=["affT"])
                    for half in range(2):
                        pt = psT[nT % 2]
                        pk = f"n_psT{nT % 2}"
                        nT += 1
                        for q in range(8):
                            dc = half * 8 + q
                            P.op("pe", lambda e, pt=pt, q=q, dc=dc, th=th: e.transpose(out=pt[:, q * 128:(q + 1) * 128], in_=hb[i][:, dc, th * 128:(th + 1) * 128], identity=g.identB[:]),
                                 reads=[f"n_hb{i}", "identB"], writes=[pk])
                        if half == 0:
                            P.op("act", lambda e, pt=pt, tc=tc: e.copy(out=htok[:, tc, 0:1024], in_=pt[:]), reads=[pk], writes=["A"])
                        else:
                            P.op("dve", lambda e, pt=pt, tc=tc: e.tensor_copy(out=htok[:, tc, 1024:2048], in_=pt[:]), reads=[pk], writes=["A"])
        P.barrier()


def stage_conv(g, l, j):
    nc, P = g.nc, g.P
    with contextlib.ExitStack() as es:
        sb = lambda name, shape, dt: es.enter_context(nc.sbuf_tensor(uname(name), shape, dt))
        hT = g.A[:, :].rearrange("p (kc t) -> p kc t", kc=KC)
        wt = [sb(f"c_wt{i}", [128, 3, KC, 128], BF16) for i in range(2)]
        cu = [sb(f"c_cu{i}", [128, T + 8], F32) for i in range(2)]
        bf = [sb(f"c_b{i}", [128, T], BF16) for i in range(2)]
        csb = [sb(f"c_c{i}", [128, 512], F32) for i in range(2)]
        z1 = sb("c_z1", [128, T], F32)
        z2 = sb("c_z2", [128, T], F32)
        zo = [sb(f"c_zo{i}", [128, T], BF16) for i in range(2)]
        ps = [[es.enter_context(nc.psum_tensor(uname(f"c_ps{i}_{k}"), [128, 512], F32)) for k in range(3)] for i in range(2)]
        LOFF, COFF = 1, 2051
        for i in range(2):
            P.op("dve", lambda e, i=i: e.memset(cu[i][:], 0.0), writes=[f"c_cu{i}"])
        win = g.conv_w_in[j].rearrange("(kc p) n -> p kc n", p=128)
        zTv = g.zT
        npi = 0
        for jc in range(KC):
            i = jc % 2
            for k in range(3):
                P.dma("pool", lambda e, k=k: e.dma_start(out=wt[i][:, k, :, :], in_=win[:, :, k * D + jc * 128:k * D + (jc + 1) * 128]),
                      writes=[f"c_wt{i}"])
            for (t0, W) in TGS:
                pi = npi % 2
                npi += 1
                for k in range(3):
                    for kc in range(KC):
                        P.op("pe", lambda e, k=k, kc=kc: e.matmul(ps[pi][k][:, 0:W], lhsT=wt[i][:, k, kc, :], rhs=hT[:, kc, t0:t0 + W],
                                                                   start=(kc == 0), stop=(kc == KC - 1)),
                             reads=[f"c_wt{i}", "A"], writes=[f"c_ps{pi}_{k}"])
                off = (LOFF + t0) if t0 < TL else (COFF + t0 - TL)
                P.op("act", lambda e: e.copy(out=bf[i][:, t0:t0 + W], in_=ps[pi][0][:, 0:W]), reads=[f"c_ps{pi}_0"], writes=[f"c_b{i}"])
                P.op("act", lambda e: e.copy(out=csb[pi][:, 0:W], in_=ps[pi][1][:, 0:W]), reads=[f"c_ps{pi}_1"], writes=[f"c_c{pi}"])
                P.op("dve", lambda e: e.tensor_tensor(out=cu[i][:, off:off + W], in0=ps[pi][2][:, 0:W], in1=csb[pi][:, 0:W], op=ALU.mult),
                     reads=[f"c_ps{pi}_2", f"c_c{pi}"], writes=[f"c_cu{i}"])
            for (t0, n, off) in ((0, TL, LOFF), (TL, TC, COFF)):
                w0 = g.convw[:, j, 0, jc:jc + 1]
                w1 = g.convw[:, j, 1, jc:jc + 1]
                w2 = g.convw[:, j, 2, jc:jc + 1]
                P.op("dve", lambda e: e.tensor_scalar_mul(out=z1[:, t0:t0 + n], in0=cu[i][:, off - 1:off - 1 + n], scalar1=w0),
                     reads=[f"c_cu{i}", "convw"], writes=["c_z1"])
                P.op("dve", lambda e: e.scalar_tensor_tensor(out=z2[:, t0:t0 + n], in0=cu[i][:, off:off + n], scalar=w1, in1=z1[:, t0:t0 + n], op0=ALU.mult, op1=ALU.add),
                     reads=[f"c_cu{i}", "convw", "c_z1"], writes=["c_z2"])
                P.op("dve", lambda e: e.scalar_tensor_tensor(out=z1[:, t0:t0 + n], in0=cu[i][:, off + 1:off + 1 + n], scalar=w2, in1=z2[:, t0:t0 + n], op0=ALU.mult, op1=ALU.add),
                     reads=[f"c_cu{i}", "convw", "c_z2"], writes=["c_z1"])
                P.op("pool", lambda e: e.tensor_tensor(out=zo[i][:, t0:t0 + n], in0=z1[:, t0:t0 + n], in1=bf[i][:, t0:t0 + n], op=ALU.mult),
                     reads=["c_z1", f"c_b{i}"], writes=[f"c_zo{i}"])
            P.dma("sp", lambda e: e.dma_start(out=zTv[jc * 128:(jc + 1) * 128, :], in_=zo[i][:]), reads=[f"c_zo{i}"], writes=["zT"])
        P.barrier()


def stage_outproj(g, l, wout):
    nc, P = g.nc, g.P
    with contextlib.ExitStack() as es:
        sb = lambda name, shape, dt: es.enter_context(nc.sbuf_tensor(uname(name), shape, dt))
        zTs = g.A[:, :].rearrange("p (kc t) -> p kc t", kc=KC)
        wt = [sb(f"o_wt{i}", [128, KC, 256], BF16) for i in range(2)]
        xb = [sb(f"o_xb{i}", [128, 512], F32) for i in range(3)]
        ps = [es.enter_context(nc.psum_tensor(uname(f"o_ps{i}"), [128, 512], F32)) for i in range(3)]
        P.dma("sp", lambda e: e.dma_start(out=zTs[:, :, :], in_=g.zT.rearrange("(kc p) t -> p kc t", p=128)), reads=["zT"], writes=["A"])
        wv = wout.rearrange("(kc p) n -> p kc n", p=128)
        n = 0
        for dq in range(8):
            i = dq % 2
            P.dma("pool", lambda e: e.dma_start(out=wt[i][:], in_=wv[:, :, dq * 256:(dq + 1) * 256]), writes=[f"o_wt{i}"])
            for dd in range(2):
                dc = dq * 2 + dd
                for (t0, W) in TGS:
                    v = var(t0)
                    pi = n % 3
                    n += 1
                    P.dma("sp", lambda e: e.dma_start(out=xb[pi][:, 0:W], in_=g.xT[dc * 128:(dc + 1) * 128, t0:t0 + W]), reads=["xT"], writes=[f"o_xb{pi}"])
                    for kc in range(KC):
                        P.op("pe", lambda e, kc=kc: e.matmul(ps[pi][:, 0:W], lhsT=wt[i][:, kc, dd * 128:(dd + 1) * 128], rhs=zTs[:, kc, t0:t0 + W],
                                                             start=(kc == 0), stop=(kc == KC - 1)),
                             reads=[f"o_wt{i}", "A"], writes=[f"o_ps{pi}"])
                    P.op("dve", lambda e: e.scalar_tensor_tensor(out=xb[pi][:, 0:W], in0=ps[pi][:, 0:W], scalar=g.cG[:, l, 0, dc, v:v + 1], in1=xb[pi][:, 0:W],
                                                                 op0=ALU.mult, op1=ALU.add),
                         reads=[f"o_ps{pi}", f"o_xb{pi}", "cG"], writes=[f"o_xb{pi}"])
                    P.dma("sp", lambda e: e.dma_start(out=g.xT[dc * 128:(dc + 1) * 128, t0:t0 + W], in_=xb[pi][:, 0:W]), reads=[f"o_xb{pi}"], writes=["xT"])
        P.barrier()


def stage_attn(g, l, j):
    nc, P = g.nc, g.P
    with contextlib.ExitStack() as es:
        sb = lambda name, shape, dt: es.enter_context(nc.sbuf_tensor(uname(name), shape, dt))
        pst = lambda name, shape, dt: es.enter_context(nc.psum_tensor(uname(name), shape, dt))
        hT = g.A[:, :].rearrange("p (kc t) -> p kc t", kc=KC)
        wq = [sb(f"t_w{i}", [128, 3, KC, 128], BF16) for i in range(2)]
        qT = [sb(f"t_q{i}", [128, T], BF16) for i in range(2)]
        kT = [sb(f"t_k{i}", [128, T], BF16) for i in range(2)]
        V = [sb(f"t_v{i}", [128, 18, 128], BF16) for i in range(2)]
        Vs = [sb(f"t_vs{i}", [128, 15, 128], BF16) for i in range(2)]
        bias = [sb(f"t_bias{i}", [64, 960], F32) for i in range(2)]
        s = [sb(f"t_s{i}", [128, 768], F32) for i in range(2)]
        p = [sb(f"t_p{i}", [128, 768], F32) for i in range(2)]
        pn = [sb(f"t_pn{i}", [128, 768], BF16) for i in range(2)]
        pT = [sb(f"t_pT{i}", [128, 6, 128], BF16) for i in range(2)]
        st = [sb(f"t_st{i}", [128, 4], F32) for i in range(2)]
        ao = [sb(f"t_ao{i}", [128, T], BF16) for i in range(2)]
        psQ = [pst(f"t_psQ{i}", [128, 512], F32) for i in range(2)]
        psS = [pst(f"t_psS{i}", [128, 512], F32) for i in range(2)]
        psC = pst("t_psC", [128, 512], F32)
        psT = [pst(f"t_psT{i}", [128, 6, 128], BF16) for i in range(2)]
        psO = pst("t_psO", [128, 512], F32)
        wv = g.attn_w_qkv[j].rearrange("(kc p) n -> p kc n", p=128)
        cnt = {"nq": 0, "nr": 0}

        def proj_chunks(hd):
            i = hd % 2
            chunks = []

            def loads():
                for k in range(3):
                    P.dma("pool", lambda e, k=k: e.dma_start(out=wq[i][:, k, :, :], in_=wv[:, :, k * D + hd * 128:k * D + (hd + 1) * 128]), writes=[f"t_w{i}"])
                P.dma("sp", lambda e: e.dma_start(out=bias[i][:], in_=g.biasT[j][hd]), writes=[f"t_bias{i}"])
            chunks.append(loads)
            for k, dst, dk in ((0, qT[i], f"t_q{i}"), (1, kT[i], f"t_k{i}")):
                for (t0, W) in TGS:
                    def qk(k=k, dst=dst, dk=dk, t0=t0, W=W):
                        pq = psQ[cnt["nq"] % 2]; pk = f"t_psQ{cnt['nq'] % 2}"; cnt["nq"] += 1
                        for kc in range(KC):
                            P.op("pe", lambda e, kc=kc: e.matmul(pq[:, 0:W], lhsT=wq[i][:, k, kc, :], rhs=hT[:, kc, t0:t0 + W], start=(kc == 0), stop=(kc == KC - 1)),
                                 reads=[f"t_w{i}", "A"], writes=[pk])
                        P.op("act", lambda e: e.copy(out=dst[:, t0:t0 + W], in_=pq[:, 0:W]), reads=[pk], writes=[dk])
                    chunks.append(qk)
            for (dstV, base, nch, dkey) in ((V[i], 0, 18, f"t_v{i}"), (Vs[i], 64, 15, f"t_vs{i}")):
                c = 0
                while c < nch:
                    n4 = min(4, nch - c)

                    def vv(dstV=dstV, base=base, dkey=dkey, c=c, n4=n4):
                        pq = psQ[cnt["nq"] % 2]; pk = f"t_psQ{cnt['nq'] % 2}"; cnt["nq"] += 1
                        for q in range(n4):
                            tok0 = base + (c + q) * 128
                            for kc in range(KC):
                                P.op("pe", lambda e, kc=kc, q=q, tok0=tok0: e.matmul(pq[:, q * 128:(q + 1) * 128], lhsT=hT[:, kc, tok0:tok0 + 128], rhs=wq[i][:, 2, kc, :],
                                                                                     start=(kc == 0), stop=(kc == KC - 1)),
                                     reads=[f"t_w{i}", "A"], writes=[pk])
                        P.op("dve", lambda e: e.tensor_copy(out=dstV[:, c:c + n4, :], in_=pq[:, 0:n4 * 128]), reads=[pk], writes=[dkey])
                    chunks.append(vv)
                    c += n4
            return chunks

        def tail_a(pi, nq_rows, ncols):
            sk, pk_, pnk, stk = f"t_s{pi}", f"t_p{pi}", f"t_pn{pi}", f"t_st{pi}"
            R = slice(0, nq_rows)
            P.op("dve", lambda e: e.reduce_max(out=st[pi][R, 0:1], in_=s[pi][R, 0:ncols], axis=AX.X), reads=[sk], writes=[stk])
            P.op("dve", lambda e: e.tensor_scalar_mul(out=st[pi][R, 1:2], in0=st[pi][R, 0:1], scalar1=-1.0), reads=[stk], writes=[stk])
            P.op("act", lambda e: e.activation(out=p[pi][R, 0:ncols], in_=s[pi][R, 0:ncols], func=AF.Exp, bias=st[pi][R, 1:2], scale=1.0, accum_out=st[pi][R, 2:3]),
                 reads=[sk, stk], writes=[pk_, stk])
            P.op("dve", lambda e: e.reciprocal(out=st[pi][R, 3:4], in_=st[pi][R, 2:3]), reads=[stk], writes=[stk])
            P.op("act", lambda e: e.activation(out=pn[pi][R, 0:ncols], in_=p[pi][R, 0:ncols], func=AF.Copy, scale=st[pi][R, 3:4]),
                 reads=[pk_, stk], writes=[pnk])

        def tail_b(i, pi, nq_rows, nkb, vchunks, out_ap, out_key):
            pnk, ptk = f"t_pn{pi}", f"t_pT{pi}"
            R = slice(0, nq_rows)
            for kb in range(nkb):
                P.op("pe", lambda e, kb=kb: e.transpose(out=psT[pi][:, kb, 0:nq_rows], in_=pn[pi][R, kb * 128:(kb + 1) * 128], identity=g.identB[0:nq_rows, 0:nq_rows]),
                     reads=[pnk, "identB"], writes=[f"t_psT{pi}"])
            P.op("act", lambda e: e.copy(out=pT[pi][:, 0:nkb, 0:nq_rows], in_=psT[pi][:, 0:nkb, 0:nq_rows]), reads=[f"t_psT{pi}"], writes=[ptk])
            for kb in range(nkb):
                P.op("pe", lambda e, kb=kb: e.matmul(psO[:, 0:nq_rows], lhsT=vchunks[kb], rhs=pT[pi][:, kb, 0:nq_rows], start=(kb == 0), stop=(kb == nkb - 1)),
                     reads=[ptk, f"t_v{i}", f"t_vs{i}"], writes=["t_psO"])
            P.op("act", lambda e: e.copy(out=out_ap, in_=psO[:, 0:nq_rows]), reads=["t_psO"], writes=[out_key])

        def lat_a(i, r, pi):
            rs = min(max(r - 4, 0), 24)
            dr0 = rs - r + 7
            P.op("pe", lambda e: e.matmul(psS[pi][0:64, 0:512], lhsT=qT[i][:, r * 64:(r + 1) * 64], rhs=kT[i][:, rs * 64:rs * 64 + 512], start=True, stop=True),
                 reads=[f"t_q{i}", f"t_k{i}"], writes=[f"t_psS{pi}"])
            P.op("pe", lambda e: e.matmul(psC[0:64, 0:256], lhsT=qT[i][:, r * 64:(r + 1) * 64], rhs=kT[i][:, TL:T], start=True, stop=True),
                 reads=[f"t_q{i}", f"t_k{i}"], writes=["t_psC"])
            P.op("dve", lambda e: e.scalar_tensor_tensor(out=s[pi][0:64, 0:512], in0=psS[pi][0:64, 0:512], scalar=SCALE, in1=bias[i][:, dr0 * 64:dr0 * 64 + 512],
                                                         op0=ALU.mult, op1=ALU.add),
                 reads=[f"t_psS{pi}", f"t_bias{i}"], writes=[f"t_s{pi}"])
            P.op("act", lambda e: e.mul(out=s[pi][0:64, 512:768], in_=psC[0:64, 0:256], mul=SCALE), reads=["t_psC"], writes=[f"t_s{pi}"])
            tail_a(pi, 64, 768)

        def lat_b(i, r, pi):
            rs = min(max(r - 4, 0), 24)
            if rs % 2 == 0:
                vch = [V[i][:, rs // 2 + kb, :] for kb in range(4)]
            else:
                vch = [Vs[i][:, (rs - 1) // 2 + kb, :] for kb in range(4)]
            vch += [V[i][:, 16, :], V[i][:, 17, :]]
            tail_b(i, pi, 64, 6, vch, ao[i][:, r * 64:(r + 1) * 64], f"t_ao{i}")

        def ctx_a(i, qc, pi):
            q0 = TL + qc * 128
            P.op("pe", lambda e: e.matmul(psS[pi][:, 0:256], lhsT=qT[i][:, q0:q0 + 128], rhs=kT[i][:, TL:T], start=True, stop=True),
                 reads=[f"t_q{i}", f"t_k{i}"], writes=[f"t_psS{pi}"])
            P.op("act", lambda e: e.mul(out=s[pi][:, 0:256], in_=psS[pi][:, 0:256], mul=SCALE), reads=[f"t_psS{pi}"], writes=[f"t_s{pi}"])
            tail_a(pi, 128, 256)

        def ctx_b(i, qc, pi):
            q0 = TL + qc * 128
            tail_b(i, pi, 128, 2, [V[i][:, 16, :], V[i][:, 17, :]], ao[i][:, q0:q0 + 128], f"t_ao{i}")

        for ch in proj_chunks(0):
            ch()
        for hd in range(NE):
            i = hd % 2
            nxt = proj_chunks(hd + 1) if hd + 1 < NE else []
            tasks = [(lat_a, lat_b, r) for r in range(32)] + [(ctx_a, ctx_b, qc) for qc in range(2)]
            prev = None
            for (fa, fb, arg) in tasks:
                pi = cnt["nr"] % 2; cnt["nr"] += 1
                fa(i, arg, pi)
                if prev is not None:
                    prev[0](i, prev[1], prev[2])
                prev = (fb, arg, pi)
                if nxt:
                    nxt.pop(0)()
            prev[0](i, prev[1], prev[2])
            while nxt:
                nxt.pop(0)()
            P.dma("sp", lambda e: e.dma_start(out=g.zT[hd * 128:(hd + 1) * 128, :], in_=ao[i][:]), reads=[f"t_ao{i}"], writes=["zT"])
        P.barrier()


def stage_topk(g, l):
    nc, P = g.nc, g.P
    with contextlib.ExitStack() as es:
        sb = lambda name, shape, dt: es.enter_context(nc.sbuf_tensor(uname(name), shape, dt))
        work = sb("k_work", [NE, TL], F32)
        workc = sb("k_workc", [NE, TC], F32)
        vals = sb("k_vals", [NE, NS], F32)
        idxu = sb("k_idxu", [NE, NS], U32)
        idxf = sb("k_idxf", [NE, NS], F32)
        ps = es.enter_context(nc.psum_tensor(uname("k_ps"), [128, 512], F32))
        P.op("dve", lambda e: e.tensor_copy(out=work[:], in_=g.affT[:, 0:TL]), reads=["affT"], writes=["k_work"])
        P.op("dve", lambda e: e.tensor_copy(out=workc[:], in_=g.affT[:, TL:T]), reads=["affT"], writes=["k_workc"])
        for (wk, wkey, nround, c0) in ((work, "k_work", CAP_L // 8, 0), (workc, "k_workc", CAP_C // 8, CAP_L)):
            for r in range(nround):
                sl = slice(c0 + r * 8, c0 + r * 8 + 8)
                P.op("dve", lambda e: e.max(out=vals[:, sl], in_=wk[:]), reads=[wkey], writes=["k_vals"])
                P.op("dve", lambda e: e.max_index(out=idxu[:, sl], in_max=vals[:, sl], in_values=wk[:]), reads=[wkey, "k_vals"], writes=["k_idxu"])
                P.op("dve", lambda e: e.match_replace(out=wk[:], in_to_replace=vals[:, sl], in_values=wk[:], imm_value=-1.0), reads=[wkey, "k_vals"], writes=[wkey])
        P.op("dve", lambda e: e.tensor_copy(out=idxf[:], in_=idxu[:]), reads=["k_idxu"], writes=["k_idxf"])
        P.dma("sp", lambda e: e.dma_start(out=g.scr_idx.rearrange("(e s) -> e s", e=NE), in_=idxf[:]), reads=["k_idxf"], writes=["scr_idx"])
        P.dma("sp", lambda e: e.dma_start(out=g.idx_bc[:], in_=g.scr_idx.partition_broadcast(128)), reads=["scr_idx"], writes=["idx_bc"])
        for ch, (c0, w) in enumerate(((0, 128), (128, 128), (256, 32))):
            P.op("pe", lambda e: e.transpose(out=ps[0:w, 0:NE], in_=idxf[:, c0:c0 + w], identity=g.identF[0:NE, 0:NE]), reads=["k_idxf", "identF"], writes=["k_ps"])
            P.op("dve", lambda e: e.tensor_copy(out=g.idx_sl[0:w, :, ch], in_=ps[0:w, 0:NE]), reads=["k_ps"], writes=["idx_sl"])
            P.op("pe", lambda e: e.transpose(out=ps[0:w, 0:NE], in_=vals[:, c0:c0 + w], identity=g.identF[0:NE, 0:NE]), reads=["k_vals", "identF"], writes=["k_ps"])
            P.op("dve", lambda e: e.tensor_copy(out=g.g_sl[0:w, :, ch], in_=ps[0:w, 0:NE]), reads=["k_ps"], writes=["g_sl"])
        P.barrier()


def stage_ffn(g, l):
    nc, P = g.nc, g.P
    with contextlib.ExitStack() as es:
        sb = lambda name, shape, dt: es.enter_context(nc.sbuf_tensor(uname(name), shape, dt))
        pst = lambda name, shape, dt: es.enter_context(nc.psum_tensor(uname(name), shape, dt))
        htok = g.A[:, :].rearrange("p (tc d) -> p tc d", tc=T // 128)
        ST = [sb(f"f_st{i}", [128, 18, 256], BF16) for i in range(2)]
        xe = [sb(f"f_xe{i}", [128, KC, NS], BF16) for i in range(2)]
        act = sb("f_act", [128, 8, NS], BF16)
        asb = [sb(f"f_a{i}", [128, NS], F32) for i in range(2)]
        w13 = [sb(f"f_w13_{i}", [128, 2, KC, 256], BF16) for i in range(2)]
        w2t = [sb(f"f_w2_{i}", [128, 8, 512], BF16) for i in range(2)]
        yt = [sb(f"f_y{i}", [128, 512], BF16) for i in range(3)]
        psG = [pst(f"f_psG{i}", [128, 512], F32) for i in range(2)]
        psA = [pst(f"f_psA{i}", [128, 512], F32) for i in range(2)]
        psU = [pst(f"f_psU{i}", [128, 512], F32) for i in range(2)]
        psY = [pst(f"f_psY{i}", [128, 512], F32) for i in range(2)]
        nw13 = 0; nw2 = 0; ny = 0; nyt = 0
        for ex in range(NE):
            i = ex % 2
            for tc in range(16):
                P.op("dve", lambda e, tc=tc: e.tensor_scalar(out=ST[i][:, tc, :], in0=g.idx_bc[:, ex * NS:ex * NS + CAP_L], scalar1=g.iotaP[:, tc:tc + 1], scalar2=None, op0=ALU.is_equal),
                     reads=["idx_bc", "iotaP"], writes=[f"f_st{i}"])
            for tc in range(2):
                P.op("dve", lambda e, tc=tc: e.tensor_scalar(out=ST[i][:, 16 + tc, 0:CAP_C], in0=g.idx_bc[:, ex * NS + CAP_L:(ex + 1) * NS], scalar1=g.iotaP[:, tc:tc + 1], scalar2=None, op0=ALU.is_equal),
                     reads=["idx_bc", "iotaP"], writes=[f"f_st{i}"])
            for dc in range(KC):
                pg = psG[dc % 2]; pk = f"f_psG{dc % 2}"
                for tc in range(16):
                    P.op("pe", lambda e, tc=tc: e.matmul(pg[:, 0:CAP_L], lhsT=htok[:, tc, dc * 128:(dc + 1) * 128], rhs=ST[i][:, tc, :], start=(tc == 0), stop=(tc == 15)),
                         reads=["A", f"f_st{i}"], writes=[pk])
                for tc in range(2):
                    P.op("pe", lambda e, tc=tc: e.matmul(pg[:, CAP_L:NS], lhsT=htok[:, 16 + tc, dc * 128:(dc + 1) * 128], rhs=ST[i][:, 16 + tc, 0:CAP_C], start=(tc == 0), stop=(tc == 1)),
                         reads=["A", f"f_st{i}"], writes=[pk])
                if dc % 2 == 0:
                    P.op("act", lambda e: e.copy(out=xe[i][:, dc, :], in_=pg[:, 0:NS]), reads=[pk], writes=[f"f_xe{i}"])
                else:
                    P.op("dve", lambda e: e.tensor_copy(out=xe[i][:, dc, :], in_=pg[:, 0:NS]), reads=[pk], writes=[f"f_xe{i}"])
            w1v = g.w1[l][ex].rearrange("(kc p) n -> p kc n", p=128)
            w3v = g.w3[l][ex].rearrange("(kc p) n -> p kc n", p=128)
            for fp in range(4):
                wi = nw13 % 2; nw13 += 1
                P.dma("pool", lambda e: e.dma_start(out=w13[wi][:, 0, :, :], in_=w1v[:, :, fp * 256:(fp + 1) * 256]), writes=[f"f_w13_{wi}"])
                P.dma("pool", lambda e: e.dma_start(out=w13[wi][:, 1, :, :], in_=w3v[:, :, fp * 256:(fp + 1) * 256]), writes=[f"f_w13_{wi}"])
                for fh in range(2):
                    fc = fp * 2 + fh
                    pa = (fp * 2 + fh) % 2
                    for kc in range(KC):
                        P.op("pe", lambda e, kc=kc: e.matmul(psA[pa][:, 0:NS], lhsT=w13[wi][:, 0, kc, fh * 128:(fh + 1) * 128], rhs=xe[i][:, kc, :], start=(kc == 0), stop=(kc == KC - 1)),
                             reads=[f"f_w13_{wi}", f"f_xe{i}"], writes=[f"f_psA{pa}"])
                    for kc in range(KC):
                        P.op("pe", lambda e, kc=kc: e.matmul(psU[pa][:, 0:NS], lhsT=w13[wi][:, 1, kc, fh * 128:(fh + 1) * 128], rhs=xe[i][:, kc, :], start=(kc == 0), stop=(kc == KC - 1)),
                             reads=[f"f_w13_{wi}", f"f_xe{i}"], writes=[f"f_psU{pa}"])
                    P.op("act", lambda e: e.activation(out=asb[pa][:], in_=psA[pa][:, 0:NS], func=AF.Silu), reads=[f"f_psA{pa}"], writes=[f"f_a{pa}"])
                    P.op("dve", lambda e: e.tensor_tensor(out=act[:, fc, :], in0=psU[pa][:, 0:NS], in1=asb[pa][:], op=ALU.mult), reads=[f"f_psU{pa}", f"f_a{pa}"], writes=[f"f_act{fc}"])
            w2v = g.w2[l][ex].rearrange("(fc p) n -> p fc n", p=128)
            for cg in range(4):
                wi = nw2 % 2; nw2 += 1
                P.dma("pool", lambda e: e.dma_start(out=w2t[wi][:], in_=w2v[:, :, cg * 512:(cg + 1) * 512]), writes=[f"f_w2_{wi}"])
                for ch, (s0, w) in enumerate(((0, 128), (128, 128), (256, 32))):
                    py = psY[ny % 2]; pk = f"f_psY{ny % 2}"; ny += 1
                    yi = nyt % 3; nyt += 1
                    for fc in range(8):
                        P.op("pe", lambda e, fc=fc: e.matmul(py[0:w, :], lhsT=act[:, fc, s0:s0 + w], rhs=w2t[wi][:, fc, :], start=(fc == 0), stop=(fc == 7)),
                             reads=[f"f_w2_{wi}"] + [f"f_act{q}" for q in range(8)], writes=[pk])
                    P.op("dve", lambda e: e.tensor_scalar_mul(out=yt[yi][0:w, :], in0=py[0:w, :], scalar1=g.g_sl[0:w, ex, ch:ch + 1]), reads=[pk, "g_sl"], writes=[f"f_y{yi}"])
                    P.dma("sp", lambda e: e.dma_start(out=g.ybuf[ex, ch, 0:w, cg * 512:(cg + 1) * 512], in_=yt[yi][0:w, :]), reads=[f"f_y{yi}"], writes=["ybuf"])
        P.barrier()


def stage_scatter(g, l):
    nc, P = g.nc, g.P
    with contextlib.ExitStack() as es:
        sb = lambda name, shape, dt: es.enter_context(nc.sbuf_tensor(uname(name), shape, dt))
        pst = lambda name, shape, dt: es.enter_context(nc.psum_tensor(uname(name), shape, dt))
        Y = g.A[:, 0:NE * 2 * 1024].rearrange("p (e c n) -> p e c n", e=NE, c=2)
        Yc = g.A[:, 0:NE * D].rearrange("p (e n) -> p e n", e=NE)
        ti = sb("s_ti", [128, TL], I32)
        iotaF = sb("s_iotaF", [128, TL], F32)
        P.op("pool", lambda e: e.iota(ti[:, :], pattern=[[1, TL]], base=0, channel_multiplier=0), writes=["s_ti"])
        P.op("dve", lambda e: e.tensor_copy(out=iotaF[:], in_=ti[:, :]), reads=["s_ti"], writes=["iotaF"])
        Soh = sb("s_oh", [128, NE * 2, 512], BF16)
        Sc = sb("s_ohc", [128, NE, TC], BF16)
        xb = [sb(f"s_xb{i}", [128, 512], F32) for i in range(3)]
        ps = [pst(f"s_ps{i}", [128, 512], F32) for i in range(3)]
        n = 0
        for dh in range(2):
            for ex in range(NE):
                P.dma("sp", lambda e, ex=ex: e.dma_start(out=Y[:, ex, :, :], in_=g.ybuf[ex, 0:2, :, dh * 1024:(dh + 1) * 1024].rearrange("c p n -> p c n")),
                      reads=["ybuf"], writes=["A"])
            for (t0, W) in TGS[:4]:
                for ex in range(NE):
                    for ch in range(2):
                        eng = "dve"
                        P.op(eng, lambda e, ex=ex, ch=ch: e.tensor_scalar(out=Soh[:, ex * 2 + ch, :], in0=iotaF[:, t0:t0 + 512], scalar1=g.idx_sl[:, ex, ch:ch + 1], scalar2=None, op0=ALU.is_equal),
                             reads=["iotaF", "idx_sl"], writes=[f"s_oh{ex}_{ch}"])
                for dcl in range(8):
                    dc = dh * 8 + dcl
                    pi = n % 3; n += 1
                    P.dma("sp", lambda e: e.dma_start(out=xb[pi][:, 0:W], in_=g.xT[dc * 128:(dc + 1) * 128, t0:t0 + W]), reads=["xT"], writes=[f"s_xb{pi}"])
                    for ex in range(NE):
                        for ch in range(2):
                            P.op("pe", lambda e, ex=ex, ch=ch: e.matmul(ps[pi][:, 0:W], lhsT=Y[:, ex, ch, dcl * 128:(dcl + 1) * 128], rhs=Soh[:, ex * 2 + ch, :],
                                                                        start=(ex == 0 and ch == 0), stop=(ex == NE - 1 and ch == 1)),
                                 reads=["A", f"s_oh{ex}_{ch}"], writes=[f"s_ps{pi}"])
                    P.op("dve", lambda e: e.scalar_tensor_tensor(out=xb[pi][:, 0:W], in0=ps[pi][:, 0:W], scalar=g.cG[:, l, 1, dc, 0:1], in1=xb[pi][:, 0:W], op0=ALU.mult, op1=ALU.add),
                         reads=[f"s_ps{pi}", f"s_xb{pi}", "cG"], writes=[f"s_xb{pi}"])
                    P.dma("sp", lambda e: e.dma_start(out=g.xT[dc * 128:(dc + 1) * 128, t0:t0 + W], in_=xb[pi][:, 0:W]), reads=[f"s_xb{pi}"], writes=["xT"])
        P.dma("sp", lambda e: e.dma_start(out=Yc[0:CAP_C, :, :], in_=g.ybuf[:, 2, 0:CAP_C, :].rearrange("e p n -> p e n")), reads=["ybuf"], writes=["A"])
        for ex in range(NE):
            P.op("dve", lambda e, ex=ex: e.tensor_scalar(out=Sc[0:CAP_C, ex, :], in0=iotaF[0:CAP_C, 0:TC], scalar1=g.idx_sl[0:CAP_C, ex, 2:3], scalar2=None, op0=ALU.is_equal),
                 reads=["iotaF", "idx_sl"], writes=["s_ohc"])
        for dc in range(KC):
            pi = n % 3; n += 1
            P.dma("sp", lambda e: e.dma_start(out=xb[pi][:, 0:TC], in_=g.xT[dc * 128:(dc + 1) * 128, TL:T]), reads=["xT"], writes=[f"s_xb{pi}"])
            for ex in range(NE):
                P.op("pe", lambda e, ex=ex: e.matmul(ps[pi][:, 0:TC], lhsT=Yc[0:CAP_C, ex, dc * 128:(dc + 1) * 128], rhs=Sc[0:CAP_C, ex, :], start=(ex == 0), stop=(ex == NE - 1)),
                     reads=["A", "s_ohc"], writes=[f"s_ps{pi}"])
            P.op("dve", lambda e: e.scalar_tensor_tensor(out=xb[pi][:, 0:TC], in0=ps[pi][:, 0:TC], scalar=g.cG[:, l, 1, dc, 1:2], in1=xb[pi][:, 0:TC], op0=ALU.mult, op1=ALU.add),
                 reads=[f"s_ps{pi}", f"s_xb{pi}", "cG"], writes=[f"s_xb{pi}"])
            P.dma("sp", lambda e: e.dma_start(out=g.xT[dc * 128:(dc + 1) * 128, TL:T], in_=xb[pi][:, 0:TC]), reads=[f"s_xb{pi}"], writes=["xT"])
        P.barrier()


def stage_final(g):
    nc, P = g.nc, g.P
    with contextlib.ExitStack() as es:
        sb = lambda name, shape, dt: es.enter_context(nc.sbuf_tensor(uname(name), shape, dt))
        W = 256
        xb = [sb(f"n_xb{i}", [128, KC, W], F32) for i in range(2)]
        sq = [sb(f"n_sq{i}", [128, KC, W], F32) for i in range(2)]
        rstd = [sb(f"n_rstd{i}", [128, W], F32) for i in range(2)]
        ot = [sb(f"f_ot{i}", [128, D], F32) for i in range(2)]
        ps = [es.enter_context(nc.psum_tensor(uname(f"n_ps{i}"), [128, 512], F32)) for i in range(2)]
        pst = [es.enter_context(nc.psum_tensor(uname(f"f_pst{i}"), [128, 512], F32)) for i in range(4)]
        tiles = (xb, sq, rstd, None, ps)
        npt = 0
        for bi, (t0, _) in enumerate(BLKS[:TL // 256]):
            i = bi % 2
            norm_block(g, tiles, 0, 0, t0, W, i)
            for kc in range(KC):
                P.op("dve", lambda e, kc=kc: e.scalar_tensor_tensor(out=sq[i][:, kc, :], in0=xb[i][:, kc, :], scalar=g.finalg[:, kc:kc + 1],
                                                                   in1=rstd[i][:, :], op0=ALU.mult, op1=ALU.mult),
                     reads=[f"n_xb{i}", f"n_rstd{i}", "finalg", f"n_ps{i}"], writes=[f"n_sq{i}"])
            for th in range(2):
                oi = (bi * 2 + th) % 2
                for q4 in range(4):
                    pp = pst[npt % 4]
                    pk = f"f_pst{npt % 4}"
                    npt += 1
                    for q in range(4):
                        dc = q4 * 4 + q
                        P.op("pe", lambda e, pp=pp, q=q, dc=dc, th=th: e.transpose(out=pp[:, q * 128:(q + 1) * 128], in_=sq[i][:, dc, th * 128:(th + 1) * 128], identity=g.identF[:]),
                             reads=[f"n_sq{i}", "identF"], writes=[pk])
                    if q4 % 2 == 0:
                        P.op("dve", lambda e, pp=pp, q4=q4: e.tensor_copy(out=ot[oi][:, q4 * 512:(q4 + 1) * 512], in_=pp[:]), reads=[pk], writes=[f"f_ot{oi}_{q4}"])
                    else:
                        P.op("act", lambda e, pp=pp, q4=q4: e.copy(out=ot[oi][:, q4 * 512:(q4 + 1) * 512], in_=pp[:]), reads=[pk], writes=[f"f_ot{oi}_{q4}"])
                r0 = t0 + th * 128
                P.dma("sp", lambda e, r0=r0, oi=oi: e.dma_start(out=g.out[r0:r0 + 128, :], in_=ot[oi][:]),
                      reads=[f"f_ot{oi}_{q}" for q in range(4)], writes=["out"])
        P.barrier()


def host_layout(inputs, b):
    f = lambda a: np.ascontiguousarray(a, dtype=np.float32)
    c = np.asarray(inputs["c"])[b]
    cc = np.asarray(inputs["c_ctx"])
    cT = np.stack([c.reshape(KC, 128).T, cc.reshape(KC, 128).T], axis=-1)
    bada = np.asarray(inputs["b_ada"]).reshape(DEPTH, 96, 128).transpose(2, 0, 1)
    ng = np.asarray(inputs["norm_g"]).reshape(DEPTH, 2, KC, 128).transpose(3, 0, 1, 2)
    fg = np.asarray(inputs["final_g"]).reshape(KC, 128).T
    cw = np.asarray(inputs["conv_w"]).reshape(2, 3, KC, 128).transpose(3, 0, 1, 2)
    return {
        "x_in": f(np.asarray(inputs["x"])[b]), "ctx_in": f(np.asarray(inputs["ctx"])[b]),
        "cT": f(cT), "badaT": f(bada), "normgT": f(ng), "finalgT": f(fg), "convwT": f(cw),
    }


def bias_table(rpb):
    rpb = np.asarray(rpb, dtype=np.float32)
    col = np.arange(64)
    cs = np.clip(col - 8, 0, 48)
    colin = (col[None, :] >= cs[:, None]) & (col[None, :] < cs[:, None] + 16)
    dci = np.clip(col[None, :] - col[:, None] + 15, 0, 30)
    tb = rpb[:, :, :, dci]
    tb = np.where(colin[None, None, None], tb, np.float32(-1e30))
    tb = tb.transpose(0, 1, 3, 2, 4).reshape(2, NE, 64, 15 * 64)
    return np.ascontiguousarray(tb, dtype=np.float32)


def shared_inputs(inputs, names):
    out = {}
    for n in names:
        if n.startswith("biasT"):
            out[n] = bias_table(inputs["attn_rpb"])[int(n[5:])]
        elif n.startswith("w1_") or n.startswith("w3_") or n.startswith("w2_"):
            out[n] = np.ascontiguousarray(np.asarray(inputs[n[:2]])[int(n[3:])], dtype=np.float32)
        else:
            base = n.rstrip("0123456789")
            out[n] = np.ascontiguousarray(np.asarray(inputs[base])[int(n[len(base):])], dtype=np.float32)
    return out


_CACHE = {}


def kernel(**inputs):
    if "nc" not in _CACHE:
        _CACHE["nc"] = build_program()
    nc, g = _CACHE["nc"]
    shared = shared_inputs(inputs, g.names)
    in_maps = []
    for core in range(4):
        m = dict(shared)
        m.update(host_layout(inputs, core))
        in_maps.append(m)
    res = run_bass_kernel_spmd(nc, in_maps, core_ids=list(range(4)))
    out = np.stack([res.results[b]["out"] for b in range(4)], axis=0)
    return out.astype(np.float32)
```

```python
import contextlib
import numpy as np
import concourse.bass as bass
import concourse.mybir as mybir
from concourse.bass_utils import run_bass_kernel_spmd

F32 = mybir.dt.float32
BF16 = mybir.dt.bfloat16
U32 = mybir.dt.uint32
I32 = mybir.dt.int32
AF = mybir.ActivationFunctionType
ALU = mybir.AluOpType
AX = mybir.AxisListType

D = 2048
TL = 2048
TC = 256
T = TL + TC
KC = 16
NE = 16
FF = 1024
DEPTH = 4
CAP_L = 256
CAP_C = 32
NS = CAP_L + CAP_C
EPS = 1e-6
TGS = [(0, 512), (512, 512), (1024, 512), (1536, 512), (2048, 256)]
BLKS = [(t0, 256) for t0 in range(0, T, 256)]
SCALE = 128 ** -0.5

COMPUTE = ("pe", "act", "dve", "pool")
ALLQ = ("pe", "act", "dve", "pool", "sp")


def var(t0):
    return 0 if t0 < TL else 1


class Prog:
    def __init__(self, nc):
        self.nc = nc
        self.eng = {"pe": nc.tensor, "act": nc.scalar, "dve": nc.vector, "pool": nc.gpsimd, "sp": nc.sync}
        self.csem = {e: nc.alloc_semaphore(name=f"cs_{e}") for e in COMPUTE}
        self.ccnt = {e: 0 for e in COMPUTE}
        self.dsem = {}
        self.dcnt = {}
        self.waited = {e: {} for e in ALLQ}
        self.res = {}
        self.sems = {f"cs_{e}": self.csem[e] for e in COMPUTE}
        self.n_instr = 0
        self.n_wait = 0
        self.dma_rr = 0

    def _deps(self, reads, writes):
        deps = []
        for r in reads:
            st = self.res.get(r)
            if st and st[0] is not None:
                deps.append(st[0])
        for w in writes:
            st = self.res.get(w)
            if st:
                if st[0] is not None:
                    deps.append(st[0])
                deps.extend(st[1])
        return deps

    def _emit_waits(self, e, deps, skip_sem=None):
        need = {}
        for (sn, v) in deps:
            if sn == skip_sem:
                continue
            if self.waited[e].get(sn, 0) >= v:
                continue
            if need.get(sn, 0) < v:
                need[sn] = v
        for sn, v in need.items():
            self.waited[e][sn] = v
            self.eng[e].wait_ge(self.sems[sn], v)
            self.n_wait += 1

    def _record(self, dep, reads, writes):
        for r in reads:
            st = self.res.setdefault(r, [None, []])
            st[1].append(dep)
        for w in writes:
            self.res[w] = [dep, []]

    def op(self, e, fn, reads=(), writes=()):
        deps = self._deps(reads, writes)
        skip = "cs_pe" if e == "pe" else None
        self._emit_waits(e, deps, skip_sem=skip)
        self.ccnt[e] += 1
        fn(self.eng[e]).then_inc(self.csem[e], 1)
        self._record((f"cs_{e}", self.ccnt[e]), reads, writes)
        self.n_instr += 1

    def dma(self, qe, fn, reads=(), writes=(), sem_key=None):
        key = sem_key if sem_key is not None else writes[0]
        if key not in self.dsem:
            name = f"ds_{len(self.dsem)}"
            h = self.nc.alloc_semaphore(name=name)
            self.dsem[key] = (h, name)
            self.sems[name] = h
            self.dcnt[name] = 0
        h, name = self.dsem[key]
        deps = self._deps(reads, writes)
        self._emit_waits(qe, deps)
        self.dcnt[name] += 16
        fn(self.eng[qe]).then_inc(h, 16)
        self._record((name, self.dcnt[name]), reads, writes)
        self.n_instr += 1

    def barrier(self):
        targets = [(f"cs_{e}", self.ccnt[e]) for e in COMPUTE] + list(self.dcnt.items())
        for e in ALLQ:
            self._emit_waits(e, targets)
        self.res.clear()


class Ctx:
    pass


_UID = [0]


def uname(name):
    _UID[0] += 1
    return f"{name}_u{_UID[0]}"


def build_program(n_layers=DEPTH, debug=False, moe=True, final=True, ada=True, x0=True, layers=None):
    nc = bass.Bass("TRN2", target_bir_lowering=False)
    g = Ctx()
    g.nc = nc
    ext = lambda name, shape, dt=F32: nc.dram_tensor(name, shape, dt, kind="ExternalInput").ap()
    g.x_in = ext("x_in", [TL, D])
    g.ctx_in = ext("ctx_in", [TC, D])
    g.cT = ext("cT", [128, KC, 2])
    g.badaT = ext("badaT", [128, DEPTH, 96])
    g.normgT = ext("normgT", [128, DEPTH, 2, KC])
    g.finalgT = ext("finalgT", [128, KC])
    g.convwT = ext("convwT", [128, 2, 3, KC])
    layers = list(range(n_layers)) if layers is None else list(layers)
    g.layers = layers
    g.names = []
    g.w_ada, g.w_router, g.w1, g.w3, g.w2 = {}, {}, {}, {}, {}
    g.conv_w_in, g.conv_w_out, g.attn_w_qkv, g.attn_w_out, g.biasT = {}, {}, {}, {}, {}
    for l in layers:
        j = l // 2
        g.w_ada[l] = ext(f"w_ada{l}", [D, 6 * D]); g.names.append(f"w_ada{l}")
        if l % 2 == 0:
            g.conv_w_in[j] = ext(f"conv_w_in{j}", [D, 3 * D]); g.conv_w_out[j] = ext(f"conv_w_out{j}", [D, D])
            g.names += [f"conv_w_in{j}", f"conv_w_out{j}"]
        else:
            g.attn_w_qkv[j] = ext(f"attn_w_qkv{j}", [D, 3 * D]); g.attn_w_out[j] = ext(f"attn_w_out{j}", [D, D])
            g.biasT[j] = ext(f"biasT{j}", [NE, 64, 960])
            g.names += [f"attn_w_qkv{j}", f"attn_w_out{j}", f"biasT{j}"]
        if moe:
            g.w_router[l] = ext(f"w_router{l}", [D, NE])
            g.w1[l] = ext(f"w1_{l}", [NE, D, FF]); g.w3[l] = ext(f"w3_{l}", [NE, D, FF]); g.w2[l] = ext(f"w2_{l}", [NE, FF, D])
            g.names += [f"w_router{l}", f"w1_{l}", f"w3_{l}", f"w2_{l}"]
    g.out = nc.dram_tensor("out", [TL, D], F32, kind="ExternalOutput").ap()
    skind = "Internal"
    g.xT = nc.dram_tensor("xT", [D, T], F32, kind=skind).ap()
    g.zT = nc.dram_tensor("zT", [D, T], BF16, kind=skind).ap()
    g.ybuf = nc.dram_tensor("ybuf", [NE, 3, 128, D], BF16, kind=skind).ap()
    g.scr_idx = nc.dram_tensor("scr_idx", [NE * NS], F32, kind=skind).ap()
    g.dbg = {}

    with nc.cleanup_on_exit():
        P = Prog(nc)
        g.P = P
        with contextlib.ExitStack() as es:
            sb = lambda name, shape, dt: es.enter_context(nc.sbuf_tensor(uname(name), shape, dt))
            g.A = sb("Abuf", [128, KC * T], BF16)
            g.identF = sb("identF", [128, 128], F32)
            g.identB = sb("identB", [128, 128], BF16)
            g.onesF = sb("onesF", [128, 128], F32)
            g.mod = sb("mod", [128, DEPTH, 96, 2], F32)
            g.cA = sb("cA", [128, DEPTH, 2, KC, 2], F32)
            g.cB = sb("cB", [128, DEPTH, 2, KC, 2], F32)
            g.cG = sb("cG", [128, DEPTH, 2, KC, 2], F32)
            g.normg = sb("normg", [128, DEPTH, 2, KC], F32)
            g.finalg = sb("finalg", [128, KC], F32)
            g.convw = sb("convw", [128, 2, 3, KC], F32)
            g.iotaP = sb("iotaP", [128, KC], F32)
            g.epsT = sb("epsT", [128, 1], F32)
            g.affT = sb("affT", [NE, T], F32)
            g.idx_bc = sb("idx_bc", [128, NE * NS], F32)
            g.idx_sl = sb("idx_sl", [128, NE, 3], F32)
            g.g_sl = sb("g_sl", [128, NE, 3], F32)
            stage_consts(g)
            if ada:
                stage_ada(g)
            if x0:
                stage_x0(g)
            for l in g.layers:
                j = l // 2
                stage_norm(g, l, 0)
                if l % 2 == 0:
                    stage_conv(g, l, j)
                    stage_outproj(g, l, g.conv_w_out[j])
                else:
                    stage_attn(g, l, j)
                    stage_outproj(g, l, g.attn_w_out[j])
                if debug:
                    snapshot(g, 2 * l)
                if moe:
                    stage_norm(g, l, 1)
                    stage_topk(g, l)
                    stage_ffn(g, l)
                    stage_scatter(g, l)
                    if debug:
                        snapshot(g, 2 * l + 1)
            if final:
                stage_final(g)
            P.barrier()
    g.stats = (P.n_instr, P.n_wait, len(P.dsem))
    return nc, g


def snapshot(g, k):
    P = g.P
    g.dbg[k] = g.nc.dram_tensor(f"dbg{k}", [D, T], F32, kind="ExternalOutput").ap()
    for q in range(4):
        P.dma("sp", lambda e, q=q: e.dma_start(out=g.dbg[k][q * 512:(q + 1) * 512, :], in_=g.xT[q * 512:(q + 1) * 512, :]), reads=["xT"], writes=[f"dbg{k}"])
    P.barrier()


def stage_consts(g):
    nc, P = g.nc, g.P
    with contextlib.ExitStack() as es:
        ti = es.enter_context(nc.sbuf_tensor(uname("c_ti"), [128, TL], I32))
        P.op("pool", lambda e: e.iota(ti[:, 0:128], pattern=[[1, 128]], base=0, channel_multiplier=-1), writes=["c_ti"])
        P.op("dve", lambda e: e.tensor_single_scalar(out=g.identF[:], in_=ti[:, 0:128], scalar=0, op=ALU.is_equal),
             reads=["c_ti"], writes=["identF"])
        P.op("dve", lambda e: e.tensor_copy(out=g.identB[:], in_=g.identF[:]), reads=["identF"], writes=["identB"])
        P.op("dve", lambda e: e.memset(g.onesF[:], 1.0), writes=["onesF"])
        P.op("dve", lambda e: e.memset(g.epsT[:], EPS), writes=["epsT"])
        P.op("pool", lambda e: e.iota(ti[:, 0:KC], pattern=[[128, KC]], base=0, channel_multiplier=1), reads=["identF"], writes=["c_ti"])
        P.op("dve", lambda e: e.tensor_copy(out=g.iotaP[:], in_=ti[:, 0:KC]), reads=["c_ti"], writes=["iotaP"])
        P.dma("sp", lambda e: e.dma_start(out=g.normg[:], in_=g.normgT), writes=["normg"])
        P.dma("sp", lambda e: e.dma_start(out=g.finalg[:], in_=g.finalgT), writes=["finalg"])
        P.dma("sp", lambda e: e.dma_start(out=g.convw[:], in_=g.convwT), writes=["convw"])
        P.barrier()


def stage_ada(g):
    nc, P = g.nc, g.P
    with contextlib.ExitStack() as es:
        sb = lambda name, shape, dt: es.enter_context(nc.sbuf_tensor(uname(name), shape, dt))
        cT = sb("a_cT", [128, KC, 2], F32)
        sig = sb("a_sig", [128, KC, 2], F32)
        csT = sb("a_csT", [128, KC, 2], BF16)
        bada = sb("a_bada", [128, DEPTH, 96], F32)
        wt = [sb(f"a_wt{i}", [128, KC, 512], BF16) for i in range(2)]
        ps = es.enter_context(nc.psum_tensor(uname("a_ps"), [128, 96, 2], F32))
        P.dma("sp", lambda e: e.dma_start(out=cT[:], in_=g.cT), writes=["a_cT"])
        P.dma("sp", lambda e: e.dma_start(out=bada[:], in_=g.badaT), writes=["a_bada"])
        P.op("act", lambda e: e.activation(out=sig[:], in_=cT[:], func=AF.Sigmoid), reads=["a_cT"], writes=["a_sig"])
        P.op("dve", lambda e: e.tensor_tensor(out=csT[:], in0=cT[:], in1=sig[:], op=ALU.mult), reads=["a_cT", "a_sig"], writes=["a_csT"])
        n = 0
        for l in g.layers:
            for pc in range(24):
                w = wt[n % 2]
                wk = f"a_wt{n % 2}"
                src = g.w_ada[l].rearrange("(kc p) n -> p kc n", p=128)[:, :, pc * 512:(pc + 1) * 512]
                P.dma("pool", lambda e, w=w, src=src: e.dma_start(out=w[:], in_=src), writes=[wk])
                for q in range(4):
                    vc = pc * 4 + q
                    for kc in range(KC):
                        P.op("pe", lambda e, w=w, q=q, kc=kc, vc=vc, l=l: e.matmul(
                            ps[:, vc, :], lhsT=w[:, kc, q * 128:(q + 1) * 128], rhs=csT[:, kc, :],
                            start=(kc == 0), stop=(kc == KC - 1)), reads=[wk, "a_csT"], writes=["a_ps"])
                n += 1
            for v in range(2):
                P.op("dve", lambda e, l=l, v=v: e.tensor_tensor(out=g.mod[:, l, :, v], in0=ps[:, :, v], in1=bada[:, l, :], op=ALU.add),
                     reads=["a_ps", "a_bada"], writes=["mod"])
            for s in range(2):
                for v in range(2):
                    sh = g.mod[:, l, (3 * s) * KC:(3 * s + 1) * KC, v]
                    sc = g.mod[:, l, (3 * s + 1) * KC:(3 * s + 2) * KC, v]
                    gt = g.mod[:, l, (3 * s + 2) * KC:(3 * s + 3) * KC, v]
                    P.op("dve", lambda e, l=l, s=s, v=v, sc=sc: e.scalar_tensor_tensor(
                        out=g.cA[:, l, s, :, v], in0=sc, scalar=1.0, in1=g.normg[:, l, s, :], op0=ALU.add, op1=ALU.mult),
                        reads=["mod", "normg"], writes=["cA"])
                    P.op("dve", lambda e, l=l, s=s, v=v, sh=sh: e.tensor_copy(out=g.cB[:, l, s, :, v], in_=sh), reads=["mod"], writes=["cB"])
                    P.op("dve", lambda e, l=l, s=s, v=v, gt=gt: e.tensor_copy(out=g.cG[:, l, s, :, v], in_=gt), reads=["mod"], writes=["cG"])
        P.barrier()


def stage_x0(g):
    nc, P = g.nc, g.P
    with contextlib.ExitStack() as es:
        sb = lambda name, shape, dt: es.enter_context(nc.sbuf_tensor(uname(name), shape, dt))
        xin = [sb(f"x0_in{i}", [128, D], F32) for i in range(2)]
        xo = [sb(f"x0_o{i}", [128, KC, 128], F32) for i in range(2)]
        ps = [es.enter_context(nc.psum_tensor(uname(f"x0_ps{i}"), [128, 4, 128], F32)) for i in range(4)]
        xTv = g.xT.rearrange("(kc p) t -> p kc t", p=128)
        pi = 0
        for tc in range(T // 128):
            i = tc % 2
            src = g.x_in[tc * 128:(tc + 1) * 128, :] if tc < 16 else g.ctx_in[(tc - 16) * 128:(tc - 15) * 128, :]
            P.dma("sp", lambda e, i=i, src=src: e.dma_start(out=xin[i][:], in_=src), writes=[f"x0_in{i}"])
            for q4 in range(4):
                pp = ps[pi % 4]
                pk = f"x0_ps{pi % 4}"
                pi += 1
                for q in range(4):
                    dc = q4 * 4 + q
                    P.op("pe", lambda e, pp=pp, q=q, dc=dc, i=i: e.transpose(out=pp[:, q, :], in_=xin[i][:, dc * 128:(dc + 1) * 128], identity=g.identF[:]),
                         reads=[f"x0_in{i}", "identF"], writes=[pk])
                eng = "dve" if q4 % 2 == 0 else "act"
                if eng == "dve":
                    P.op("dve", lambda e, pp=pp, q4=q4, i=i: e.tensor_copy(out=xo[i][:, q4 * 4:(q4 + 1) * 4, :], in_=pp[:]), reads=[pk], writes=[f"x0_o{i}_{q4}"])
                else:
                    P.op("act", lambda e, pp=pp, q4=q4, i=i: e.copy(out=xo[i][:, q4 * 4:(q4 + 1) * 4, :], in_=pp[:]), reads=[pk], writes=[f"x0_o{i}_{q4}"])
            P.dma("sp", lambda e, i=i, tc=tc: e.dma_start(out=xTv[:, :, tc * 128:(tc + 1) * 128], in_=xo[i][:]),
                  reads=[f"x0_o{i}_{q}" for q in range(4)], writes=["xT"])
        P.barrier()


def norm_block(g, es_tiles, l, s, t0, W, i, want_f32=None):
    nc, P = g.nc, g.P
    xb, sq, rstd, tmp, ps = es_tiles
    v = var(t0)
    xTv = g.xT.rearrange("(kc p) t -> p kc t", p=128)
    P.dma("sp", lambda e: e.dma_start(out=xb[i][:, :, 0:W], in_=xTv[:, :, t0:t0 + W]), reads=["xT"], writes=[f"n_xb{i}"])
    P.op("act", lambda e: e.activation(out=sq[i][:, :, 0:W], in_=xb[i][:, :, 0:W], func=AF.Square), reads=[f"n_xb{i}"], writes=[f"n_sq{i}"])
    for kc in range(KC):
        P.op("pe", lambda e, kc=kc: e.matmul(ps[i][:, 0:W], lhsT=g.onesF[:], rhs=sq[i][:, kc, 0:W], start=(kc == 0), stop=(kc == KC - 1)),
             reads=[f"n_sq{i}", "onesF"], writes=[f"n_ps{i}"])
    P.op("act", lambda e: e.activation(out=rstd[i][:, 0:W], in_=ps[i][:, 0:W], func=AF.Sqrt, bias=g.epsT[:, 0:1], scale=1.0 / D),
         reads=[f"n_ps{i}", "epsT"], writes=[f"n_rstd{i}"])
    P.op("dve", lambda e: e.reciprocal(out=rstd[i][:, 0:W], in_=rstd[i][:, 0:W]), reads=[f"n_rstd{i}"], writes=[f"n_rstd{i}"])
    return v


def stage_norm(g, l, s):
    nc, P = g.nc, g.P
    with contextlib.ExitStack() as es:
        sb = lambda name, shape, dt: es.enter_context(nc.sbuf_tensor(uname(name), shape, dt))
        W = 256
        xb = [sb(f"n_xb{i}", [128, KC, W], F32) for i in range(2)]
        sq = [sb(f"n_sq{i}", [128, KC, W], F32) for i in range(2)]
        rstd = [sb(f"n_rstd{i}", [128, W], F32) for i in range(2)]
        ps = [es.enter_context(nc.psum_tensor(uname(f"n_ps{i}"), [128, 512], F32)) for i in range(2)]
        tiles = (xb, sq, rstd, None, ps)
        if s == 0:
            hT = g.A[:, :].rearrange("p (kc t) -> p kc t", kc=KC)
        else:
            htok = g.A[:, :].rearrange("p (tc d) -> p tc d", tc=T // 128)
            hb = [sb(f"n_hb{i}", [128, KC, W], BF16) for i in range(2)]
            wr = sb("n_wr", [128, KC, NE], F32)
            lg = [sb(f"n_lg{i}", [128, NE], F32) for i in range(2)]
            st = [sb(f"n_st{i}", [128, 4], F32) for i in range(2)]
            g_affT = g.affT
            psT = [es.enter_context(nc.psum_tensor(uname(f"n_psT{i}"), [128, 1024], BF16)) for i in range(2)]
            psR = [es.enter_context(nc.psum_tensor(uname(f"n_psR{i}"), [128, 512], F32)) for i in range(2)]
            P.dma("sp", lambda e: e.dma_start(out=wr[:], in_=g.w_router[l].rearrange("(kc p) n -> p kc n", p=128)), writes=["n_wr"])
        nT = 0
        for bi, (t0, _) in enumerate(BLKS):
            i = bi % 2
            v = norm_block(g, tiles, l, s, t0, W, i)
            for kc in range(KC):
                P.op("dve", lambda e, kc=kc: e.scalar_tensor_tensor(out=sq[i][:, kc, :], in0=xb[i][:, kc, :], scalar=g.cA[:, l, s, kc, v:v + 1],
                                                                   in1=rstd[i][:, :], op0=ALU.mult, op1=ALU.mult),
                     reads=[f"n_xb{i}", f"n_rstd{i}", "cA", f"n_ps{i}"], writes=[f"n_sq{i}"])
            if s == 0:
                for kc in range(KC):
                    P.op("act", lambda e, kc=kc: e.activation(out=hT[:, kc, t0:t0 + W], in_=sq[i][:, kc, :], func=AF.Identity,
                                                              bias=g.cB[:, l, s, kc, v:v + 1], scale=1.0),
                         reads=[f"n_sq{i}", "cB"], writes=["A"])
            else:
                for kc in range(KC):
                    P.op("act", lambda e, kc=kc: e.activation(out=sq[i][:, kc, :], in_=sq[i][:, kc, :], func=AF.Identity,
                                                              bias=g.cB[:, l, s, kc, v:v + 1], scale=1.0),
                         reads=[f"n_sq{i}", "cB"], writes=[f"n_sq{i}"])
                P.op("pool", lambda e: e.tensor_copy(out=hb[i][:], in_=sq[i][:]), reads=[f"n_sq{i}"], writes=[f"n_hb{i}"])
                for th in range(2):
                    tc = (t0 + th * 128) // 128
                    for kc in range(KC):
                        P.op("pe", lambda e, kc=kc, th=th: e.matmul(psR[i][:, th * 16:(th + 1) * 16], lhsT=sq[i][:, kc, th * 128:(th + 1) * 128], rhs=wr[:, kc, :],
                                                                    start=(kc == 0), stop=(kc == KC - 1)),
                             reads=[f"n_sq{i}", "n_wr"], writes=[f"n_psR{i}a"])
                    lgi = lg[i]
                    sti = st[i]
                    P.op("dve", lambda e, th=th: e.reduce_max(out=sti[:, 0:1], in_=psR[i][:, th * 16:(th + 1) * 16], axis=AX.X),
                         reads=[f"n_psR{i}a"], writes=[f"n_st{i}"])
                    P.op("dve", lambda e: e.tensor_scalar_mul(out=sti[:, 1:2], in0=sti[:, 0:1], scalar1=-1.0), reads=[f"n_st{i}"], writes=[f"n_st{i}"])
                    P.op("act", lambda e, th=th: e.activation(out=lgi[:], in_=psR[i][:, th * 16:(th + 1) * 16], func=AF.Exp, bias=sti[:, 1:2], scale=1.0,
                                                              accum_out=sti[:, 2:3]),
                         reads=[f"n_psR{i}a", f"n_st{i}"], writes=[f"n_lg{i}", f"n_st{i}"])
                    P.op("dve", lambda e: e.reciprocal(out=sti[:, 3:4], in_=sti[:, 2:3]), reads=[f"n_st{i}"], writes=[f"n_st{i}"])
                    P.op("dve", lambda e: e.tensor_scalar_mul(out=lgi[:], in0=lgi[:], scalar1=sti[:, 3:4]), reads=[f"n_st{i}", f"n_lg{i}"], writes=[f"n_lg{i}"])
                    P.op("pe", lambda e: e.transpose(out=psR[i][0:16, 128:256], in_=lgi[:], identity=g.identF[:]),
                         reads=[f"n_lg{i}", "identF"], writes=[f"n_psR{i}b"])
                    P.op("act", lambda e, tc=tc: e.copy(out=g_affT[:, tc * 128:(tc + 1) * 128], in_=psR[i][0:16, 128:256]),
                         reads=[f"n_psR{i}b"], writes=["affT"])
                    for half in range(2):
                        pt = psT[nT % 2]
                        pk = f"n_psT{nT % 2}"
                        nT += 1
                        for q in range(8):
                            dc = half * 8 + q
                            P.op("pe", lambda e, pt=pt, q=q, dc=dc, th=th: e.transpose(out=pt[:, q * 128:(q + 1) * 128], in_=hb[i][:, dc, th * 128:(th + 1) * 128], identity=g.identB[:]),
                                 reads=[f"n_hb{i}", "identB"], writes=[pk])
                        if half == 0:
                            P.op("act", lambda e, pt=pt, tc=tc: e.copy(out=htok[:, tc, 0:1024], in_=pt[:]), reads=[pk], writes=["A"])
                        else:
                            P.op("dve", lambda e, pt=pt, tc=tc: e.tensor_copy(out=htok[:, tc, 1024:2048], in_=pt[:]), reads=[pk], writes=["A"])
        P.barrier()


def stage_conv(g, l, j):
    nc, P = g.nc, g.P
    with contextlib.ExitStack() as es:
        sb = lambda name, shape, dt: es.enter_context(nc.sbuf_tensor(uname(name), shape, dt))
        hT = g.A[:, :].rearrange("p (kc t) -> p kc t", kc=KC)
        wt = [sb(f"c_wt{i}", [128, 3, KC, 128], BF16) for i in range(2)]
        cu = [sb(f"c_cu{i}", [128, T + 8], F32) for i in range(2)]
        bf = [sb(f"c_b{i}", [128, T], BF16) for i in range(2)]
        csb = [sb(f"c_c{i}", [128, 512], F32) for i in range(2)]
        z1 = sb("c_z1", [128, T], F32)
        z2 = sb("c_z2", [128, T], F32)
        zo = [sb(f"c_zo{i}", [128, T], BF16) for i in range(2)]
        ps = [[es.enter_context(nc.psum_tensor(uname(f"c_ps{i}_{k}"), [128, 512], F32)) for k in range(3)] for i in range(2)]
        LOFF, COFF = 1, 2051
        for i in range(2):
            P.op("dve", lambda e, i=i: e.memset(cu[i][:], 0.0), writes=[f"c_cu{i}"])
        win = g.conv_w_in[j].rearrange("(kc p) n -> p kc n", p=128)
        zTv = g.zT
        npi = 0
        for jc in range(KC):
            i = jc % 2
            for k in range(3):
                P.dma("pool", lambda e, k=k: e.dma_start(out=wt[i][:, k, :, :], in_=win[:, :, k * D + jc * 128:k * D + (jc + 1) * 128]),
                      writes=[f"c_wt{i}"])
            for (t0, W) in TGS:
                pi = npi % 2
                npi += 1
                for k in range(3):
                    for kc in range(KC):
                        P.op("pe", lambda e, k=k, kc=kc: e.matmul(ps[pi][k][:, 0:W], lhsT=wt[i][:, k, kc, :], rhs=hT[:, kc, t0:t0 + W],
                                                                   start=(kc == 0), stop=(kc == KC - 1)),
                             reads=[f"c_wt{i}", "A"], writes=[f"c_ps{pi}_{k}"])
                off = (LOFF + t0) if t0 < TL else (COFF + t0 - TL)
                P.op("act", lambda e: e.copy(out=bf[i][:, t0:t0 + W], in_=ps[pi][0][:, 0:W]), reads=[f"c_ps{pi}_0"], writes=[f"c_b{i}"])
                P.op("act", lambda e: e.copy(out=csb[pi][:, 0:W], in_=ps[pi][1][:, 0:W]), reads=[f"c_ps{pi}_1"], writes=[f"c_c{pi}"])
                P.op("dve", lambda e: e.tensor_tensor(out=cu[i][:, off:off + W], in0=ps[pi][2][:, 0:W], in1=csb[pi][:, 0:W], op=ALU.mult),
                     reads=[f"c_ps{pi}_2", f"c_c{pi}"], writes=[f"c_cu{i}"])
            for (t0, n, off) in ((0, TL, LOFF), (TL, TC, COFF)):
                w0 = g.convw[:, j, 0, jc:jc + 1]
                w1 = g.convw[:, j, 1, jc:jc + 1]
                w2 = g.convw[:, j, 2, jc:jc + 1]
                P.op("dve", lambda e: e.tensor_scalar_mul(out=z1[:, t0:t0 + n], in0=cu[i][:, off - 1:off - 1 + n], scalar1=w0),
                     reads=[f"c_cu{i}", "convw"], writes=["c_z1"])
                P.op("dve", lambda e: e.scalar_tensor_tensor(out=z2[:, t0:t0 + n], in0=cu[i][:, off:off + n], scalar=w1, in1=z1[:, t0:t0 + n], op0=ALU.mult, op1=ALU.add),
                     reads=[f"c_cu{i}", "convw", "c_z1"], writes=["c_z2"])
                P.op("dve", lambda e: e.scalar_tensor_tensor(out=z1[:, t0:t0 + n], in0=cu[i][:, off + 1:off + 1 + n], scalar=w2, in1=z2[:, t0:t0 + n], op0=ALU.mult, op1=ALU.add),
                     reads=[f"c_cu{i}", "convw", "c_z2"], writes=["c_z1"])
                P.op("pool", lambda e: e.tensor_tensor(out=zo[i][:, t0:t0 + n], in0=z1[:, t0:t0 + n], in1=bf[i][:, t0:t0 + n], op=ALU.mult),
                     reads=["c_z1", f"c_b{i}"], writes=[f"c_zo{i}"])
            P.dma("sp", lambda e: e.dma_start(out=zTv[jc * 128:(jc + 1) * 128, :], in_=zo[i][:]), reads=[f"c_zo{i}"], writes=["zT"])
        P.barrier()


def stage_outproj(g, l, wout):
    nc, P = g.nc, g.P
    with contextlib.ExitStack() as es:
        sb = lambda name, shape, dt: es.enter_context(nc.sbuf_tensor(uname(name), shape, dt))
        zTs = g.A[:, :].rearrange("p (kc t) -> p kc t", kc=KC)
        wt = [sb(f"o_wt{i}", [128, KC, 256], BF16) for i in range(2)]
        xb = [sb(f"o_xb{i}", [128, 512], F32) for i in range(3)]
        ps = [es.enter_context(nc.psum_tensor(uname(f"o_ps{i}"), [128, 512], F32)) for i in range(3)]
        P.dma("sp", lambda e: e.dma_start(out=zTs[:, :, :], in_=g.zT.rearrange("(kc p) t -> p kc t", p=128)), reads=["zT"], writes=["A"])
        wv = wout.rearrange("(kc p) n -> p kc n", p=128)
        n = 0
        for dq in range(8):
            i = dq % 2
            P.dma("pool", lambda e: e.dma_start(out=wt[i][:], in_=wv[:, :, dq * 256:(dq + 1) * 256]), writes=[f"o_wt{i}"])
            for dd in range(2):
                dc = dq * 2 + dd
                for (t0, W) in TGS:
                    v = var(t0)
                    pi = n % 3
                    n += 1
                    P.dma("sp", lambda e: e.dma_start(out=xb[pi][:, 0:W], in_=g.xT[dc * 128:(dc + 1) * 128, t0:t0 + W]), reads=["xT"], writes=[f"o_xb{pi}"])
                    for kc in range(KC):
                        P.op("pe", lambda e, kc=kc: e.matmul(ps[pi][:, 0:W], lhsT=wt[i][:, kc, dd * 128:(dd + 1) * 128], rhs=zTs[:, kc, t0:t0 + W],
                                                             start=(kc == 0), stop=(kc == KC - 1)),
                             reads=[f"o_wt{i}", "A"], writes=[f"o_ps{pi}"])
                    P.op("dve", lambda e: e.scalar_tensor_tensor(out=xb[pi][:, 0:W], in0=ps[pi][:, 0:W], scalar=g.cG[:, l, 0, dc, v:v + 1], in1=xb[pi][:, 0:W],
                                                                 op0=ALU.mult, op1=ALU.add),
                         reads=[f"o_ps{pi}", f"o_xb{pi}", "cG"], writes=[f"o_xb{pi}"])
                    P.dma("sp", lambda e: e.dma_start(out=g.xT[dc * 128:(dc + 1) * 128, t0:t0 + W], in_=xb[pi][:, 0:W]), reads=[f"o_xb{pi}"], writes=["xT"])
        P.barrier()


def stage_attn(g, l, j):
    nc, P = g.nc, g.P
    with contextlib.ExitStack() as es:
        sb = lambda name, shape, dt: es.enter_context(nc.sbuf_tensor(uname(name), shape, dt))
        pst = lambda name, shape, dt: es.enter_context(nc.psum_tensor(uname(name), shape, dt))
        hT = g.A[:, :].rearrange("p (kc t) -> p kc t", kc=KC)
        wq = [sb(f"t_w{i}", [128, 3, KC, 128], BF16) for i in range(2)]
        qT = [sb(f"t_q{i}", [128, T], BF16) for i in range(2)]
        kT = [sb(f"t_k{i}", [128, T], BF16) for i in range(2)]
        V = sb("t_v", [128, 18, 128], BF16)
        Vs = sb("t_vs", [128, 16, 128], BF16)
        bias = [sb(f"t_bias{i}", [64, 960], F32) for i in range(2)]
        s = [sb(f"t_s{i}", [128, 768], F32) for i in range(2)]
        p = [sb(f"t_p{i}", [128, 768], F32) for i in range(2)]
        pn = [sb(f"t_pn{i}", [128, 768], BF16) for i in range(2)]
        pT = [sb(f"t_pT{i}", [128, 6, 128], BF16) for i in range(2)]
        st = [sb(f"t_st{i}", [128, 4], F32) for i in range(2)]
        ao = [sb(f"t_ao{i}", [128, T], BF16) for i in range(2)]
        psQ = [pst(f"t_psQ{i}", [128, 512], F32) for i in range(2)]
        psS = [pst(f"t_psS{i}", [128, 512], F32) for i in range(2)]
        psC = pst("t_psC", [128, 512], F32)
        psT = [pst(f"t_psT{i}", [128, 6, 128], BF16) for i in range(2)]
        psO = pst("t_psO", [128, 512], F32)
        wv = g.attn_w_qkv[j].rearrange("(kc p) n -> p kc n", p=128)
        nq = 0
        nr = 0
        for hd in range(NE):
            i = hd % 2
            for k in range(3):
                P.dma("pool", lambda e, k=k: e.dma_start(out=wq[i][:, k, :, :], in_=wv[:, :, k * D + hd * 128:k * D + (hd + 1) * 128]), writes=[f"t_w{i}"])
            P.dma("sp", lambda e: e.dma_start(out=bias[i][:], in_=g.biasT[j][hd]), writes=[f"t_bias{i}"])
            for k, dst, dk in ((0, qT[i], f"t_q{i}"), (1, kT[i], f"t_k{i}")):
                for (t0, W) in TGS:
                    pq = psQ[nq % 2]; pk = f"t_psQ{nq % 2}"; nq += 1
                    for kc in range(KC):
                        P.op("pe", lambda e, kc=kc: e.matmul(pq[:, 0:W], lhsT=wq[i][:, k, kc, :], rhs=hT[:, kc, t0:t0 + W], start=(kc == 0), stop=(kc == KC - 1)),
                             reads=[f"t_w{i}", "A"], writes=[pk])
                    P.op("act", lambda e: e.copy(out=dst[:, t0:t0 + W], in_=pq[:, 0:W]), reads=[pk], writes=[dk])
            for (dstV, base, nch, dkey) in ((V, 0, 18, "t_v"), (Vs, 64, 15, "t_vs")):
                c = 0
                while c < nch:
                    n4 = min(4, nch - c)
                    pq = psQ[nq % 2]; pk = f"t_psQ{nq % 2}"; nq += 1
                    for q in range(n4):
                        tok0 = base + (c + q) * 128
                        for kc in range(KC):
                            P.op("pe", lambda e, kc=kc, q=q, tok0=tok0: e.matmul(pq[:, q * 128:(q + 1) * 128], lhsT=hT[:, kc, tok0:tok0 + 128], rhs=wq[i][:, 2, kc, :],
                                                                                 start=(kc == 0), stop=(kc == KC - 1)),
                                 reads=[f"t_w{i}", "A"], writes=[pk])
                    P.op("dve", lambda e, c=c, n4=n4: e.tensor_copy(out=dstV[:, c:c + n4, :], in_=pq[:, 0:n4 * 128]), reads=[pk], writes=[dkey])
                    c += n4

            def tail_a(pi, nq_rows, ncols):
                sk, pk_, pnk, stk = f"t_s{pi}", f"t_p{pi}", f"t_pn{pi}", f"t_st{pi}"
                R = slice(0, nq_rows)
                P.op("dve", lambda e: e.reduce_max(out=st[pi][R, 0:1], in_=s[pi][R, 0:ncols], axis=AX.X), reads=[sk], writes=[stk])
                P.op("dve", lambda e: e.tensor_scalar_mul(out=st[pi][R, 1:2], in0=st[pi][R, 0:1], scalar1=-1.0), reads=[stk], writes=[stk])
                P.op("act", lambda e: e.activation(out=p[pi][R, 0:ncols], in_=s[pi][R, 0:ncols], func=AF.Exp, bias=st[pi][R, 1:2], scale=1.0, accum_out=st[pi][R, 2:3]),
                     reads=[sk, stk], writes=[pk_, stk])
                P.op("dve", lambda e: e.reciprocal(out=st[pi][R, 3:4], in_=st[pi][R, 2:3]), reads=[stk], writes=[stk])
                P.op("dve", lambda e: e.tensor_scalar_mul(out=pn[pi][R, 0:ncols], in0=p[pi][R, 0:ncols], scalar1=st[pi][R, 3:4]),
                     reads=[pk_, stk], writes=[pnk])

            def tail_b(pi, nq_rows, nkb, vchunks, out_ap, out_key):
                pnk, ptk = f"t_pn{pi}", f"t_pT{pi}"
                R = slice(0, nq_rows)
                for kb in range(nkb):
                    P.op("pe", lambda e, kb=kb: e.transpose(out=psT[pi][:, kb, 0:nq_rows], in_=pn[pi][R, kb * 128:(kb + 1) * 128], identity=g.identB[0:nq_rows, 0:nq_rows]),
                         reads=[pnk, "identB"], writes=[f"t_psT{pi}"])
                P.op("act", lambda e: e.copy(out=pT[pi][:, 0:nkb, 0:nq_rows], in_=psT[pi][:, 0:nkb, 0:nq_rows]), reads=[f"t_psT{pi}"], writes=[ptk])
                for kb in range(nkb):
                    P.op("pe", lambda e, kb=kb: e.matmul(psO[:, 0:nq_rows], lhsT=vchunks[kb], rhs=pT[pi][:, kb, 0:nq_rows], start=(kb == 0), stop=(kb == nkb - 1)),
                         reads=[ptk, "t_v", "t_vs"], writes=["t_psO"])
                P.op("act", lambda e: e.copy(out=out_ap, in_=psO[:, 0:nq_rows]), reads=["t_psO"], writes=[out_key])

            def lat_a(r, pi):
                rs = min(max(r - 4, 0), 24)
                dr0 = rs - r + 7
                P.op("pe", lambda e: e.matmul(psS[pi][0:64, 0:512], lhsT=qT[i][:, r * 64:(r + 1) * 64], rhs=kT[i][:, rs * 64:rs * 64 + 512], start=True, stop=True),
                     reads=[f"t_q{i}", f"t_k{i}"], writes=[f"t_psS{pi}"])
                P.op("pe", lambda e: e.matmul(psC[0:64, 0:256], lhsT=qT[i][:, r * 64:(r + 1) * 64], rhs=kT[i][:, TL:T], start=True, stop=True),
                     reads=[f"t_q{i}", f"t_k{i}"], writes=["t_psC"])
                P.op("dve", lambda e: e.scalar_tensor_tensor(out=s[pi][0:64, 0:512], in0=psS[pi][0:64, 0:512], scalar=SCALE, in1=bias[i][:, dr0 * 64:dr0 * 64 + 512],
                                                             op0=ALU.mult, op1=ALU.add),
                     reads=[f"t_psS{pi}", f"t_bias{i}"], writes=[f"t_s{pi}"])
                P.op("act", lambda e: e.mul(out=s[pi][0:64, 512:768], in_=psC[0:64, 0:256], mul=SCALE), reads=["t_psC"], writes=[f"t_s{pi}"])
                tail_a(pi, 64, 768)

            def lat_b(r, pi):
                rs = min(max(r - 4, 0), 24)
                if rs % 2 == 0:
                    vch = [V[:, rs // 2 + kb, :] for kb in range(4)]
                else:
                    vch = [Vs[:, (rs - 1) // 2 + kb, :] for kb in range(4)]
                vch += [V[:, 16, :], V[:, 17, :]]
                tail_b(pi, 64, 6, vch, ao[i][:, r * 64:(r + 1) * 64], f"t_ao{i}")

            def ctx_a(qc, pi):
                q0 = TL + qc * 128
                P.op("pe", lambda e: e.matmul(psS[pi][:, 0:256], lhsT=qT[i][:, q0:q0 + 128], rhs=kT[i][:, TL:T], start=True, stop=True),
                     reads=[f"t_q{i}", f"t_k{i}"], writes=[f"t_psS{pi}"])
                P.op("act", lambda e: e.mul(out=s[pi][:, 0:256], in_=psS[pi][:, 0:256], mul=SCALE), reads=[f"t_psS{pi}"], writes=[f"t_s{pi}"])
                tail_a(pi, 128, 256)

            def ctx_b(qc, pi):
                q0 = TL + qc * 128
                tail_b(pi, 128, 2, [V[:, 16, :], V[:, 17, :]], ao[i][:, q0:q0 + 128], f"t_ao{i}")

            tasks = [(lat_a, lat_b, r) for r in range(32)] + [(ctx_a, ctx_b, qc) for qc in range(2)]
            prev = None
            for (fa, fb, arg) in tasks:
                pi = nr % 2; nr += 1
                fa(arg, pi)
                if prev is not None:
                    prev[0](prev[1], prev[2])
                prev = (fb, arg, pi)
            prev[0](prev[1], prev[2])
            P.dma("sp", lambda e: e.dma_start(out=g.zT[hd * 128:(hd + 1) * 128, :], in_=ao[i][:]), reads=[f"t_ao{i}"], writes=["zT"])
        P.barrier()


def stage_topk(g, l):
    nc, P = g.nc, g.P
    with contextlib.ExitStack() as es:
        sb = lambda name, shape, dt: es.enter_context(nc.sbuf_tensor(uname(name), shape, dt))
        work = sb("k_work", [NE, TL], F32)
        workc = sb("k_workc", [NE, TC], F32)
        vals = sb("k_vals", [NE, NS], F32)
        idxu = sb("k_idxu", [NE, NS], U32)
        idxf = sb("k_idxf", [NE, NS], F32)
        ps = es.enter_context(nc.psum_tensor(uname("k_ps"), [128, 512], F32))
        P.op("dve", lambda e: e.tensor_copy(out=work[:], in_=g.affT[:, 0:TL]), reads=["affT"], writes=["k_work"])
        P.op("dve", lambda e: e.tensor_copy(out=workc[:], in_=g.affT[:, TL:T]), reads=["affT"], writes=["k_workc"])
        for (wk, wkey, nround, c0) in ((work, "k_work", CAP_L // 8, 0), (workc, "k_workc", CAP_C // 8, CAP_L)):
            for r in range(nround):
                sl = slice(c0 + r * 8, c0 + r * 8 + 8)
                P.op("dve", lambda e: e.max(out=vals[:, sl], in_=wk[:]), reads=[wkey], writes=["k_vals"])
                P.op("dve", lambda e: e.max_index(out=idxu[:, sl], in_max=vals[:, sl], in_values=wk[:]), reads=[wkey, "k_vals"], writes=["k_idxu"])
                P.op("dve", lambda e: e.match_replace(out=wk[:], in_to_replace=vals[:, sl], in_values=wk[:], imm_value=-1.0), reads=[wkey, "k_vals"], writes=[wkey])
        P.op("dve", lambda e: e.tensor_copy(out=idxf[:], in_=idxu[:]), reads=["k_idxu"], writes=["k_idxf"])
        P.dma("sp", lambda e: e.dma_start(out=g.scr_idx.rearrange("(e s) -> e s", e=NE), in_=idxf[:]), reads=["k_idxf"], writes=["scr_idx"])
        P.dma("sp", lambda e: e.dma_start(out=g.idx_bc[:], in_=g.scr_idx.partition_broadcast(128)), reads=["scr_idx"], writes=["idx_bc"])
        for ch, (c0, w) in enumerate(((0, 128), (128, 128), (256, 32))):
            P.op("pe", lambda e: e.transpose(out=ps[0:w, 0:NE], in_=idxf[:, c0:c0 + w], identity=g.identF[0:NE, 0:NE]), reads=["k_idxf", "identF"], writes=["k_ps"])
            P.op("dve", lambda e: e.tensor_copy(out=g.idx_sl[0:w, :, ch], in_=ps[0:w, 0:NE]), reads=["k_ps"], writes=["idx_sl"])
            P.op("pe", lambda e: e.transpose(out=ps[0:w, 0:NE], in_=vals[:, c0:c0 + w], identity=g.identF[0:NE, 0:NE]), reads=["k_vals", "identF"], writes=["k_ps"])
            P.op("dve", lambda e: e.tensor_copy(out=g.g_sl[0:w, :, ch], in_=ps[0:w, 0:NE]), reads=["k_ps"], writes=["g_sl"])
        P.barrier()


def stage_ffn(g, l):
    nc, P = g.nc, g.P
    with contextlib.ExitStack() as es:
        sb = lambda name, shape, dt: es.enter_context(nc.sbuf_tensor(uname(name), shape, dt))
        pst = lambda name, shape, dt: es.enter_context(nc.psum_tensor(uname(name), shape, dt))
        htok = g.A[:, :].rearrange("p (tc d) -> p tc d", tc=T // 128)
        ST = [sb(f"f_st{i}", [128, 18, 256], BF16) for i in range(2)]
        xe = [sb(f"f_xe{i}", [128, KC, NS], BF16) for i in range(2)]
        act = sb("f_act", [128, 8, NS], BF16)
        asb = [sb(f"f_a{i}", [128, NS], F32) for i in range(2)]
        w13 = [sb(f"f_w13_{i}", [128, 2, KC, 256], BF16) for i in range(2)]
        w2t = [sb(f"f_w2_{i}", [128, 8, 512], BF16) for i in range(2)]
        yt = [sb(f"f_y{i}", [128, 512], BF16) for i in range(3)]
        psG = [pst(f"f_psG{i}", [128, 512], F32) for i in range(2)]
        psA = [pst(f"f_psA{i}", [128, 512], F32) for i in range(2)]
        psU = [pst(f"f_psU{i}", [128, 512], F32) for i in range(2)]
        psY = [pst(f"f_psY{i}", [128, 512], F32) for i in range(2)]
        nw13 = 0; nw2 = 0; ny = 0; nyt = 0
        for ex in range(NE):
            i = ex % 2
            for tc in range(16):
                P.op("dve", lambda e, tc=tc: e.tensor_scalar(out=ST[i][:, tc, :], in0=g.idx_bc[:, ex * NS:ex * NS + CAP_L], scalar1=g.iotaP[:, tc:tc + 1], scalar2=None, op0=ALU.is_equal),
                     reads=["idx_bc", "iotaP"], writes=[f"f_st{i}"])
            for tc in range(2):
                P.op("dve", lambda e, tc=tc: e.tensor_scalar(out=ST[i][:, 16 + tc, 0:CAP_C], in0=g.idx_bc[:, ex * NS + CAP_L:(ex + 1) * NS], scalar1=g.iotaP[:, tc:tc + 1], scalar2=None, op0=ALU.is_equal),
                     reads=["idx_bc", "iotaP"], writes=[f"f_st{i}"])
            for dc in range(KC):
                pg = psG[dc % 2]; pk = f"f_psG{dc % 2}"
                for tc in range(16):
                    P.op("pe", lambda e, tc=tc: e.matmul(pg[:, 0:CAP_L], lhsT=htok[:, tc, dc * 128:(dc + 1) * 128], rhs=ST[i][:, tc, :], start=(tc == 0), stop=(tc == 15)),
                         reads=["A", f"f_st{i}"], writes=[pk])
                for tc in range(2):
                    P.op("pe", lambda e, tc=tc: e.matmul(pg[:, CAP_L:NS], lhsT=htok[:, 16 + tc, dc * 128:(dc + 1) * 128], rhs=ST[i][:, 16 + tc, 0:CAP_C], start=(tc == 0), stop=(tc == 1)),
                         reads=["A", f"f_st{i}"], writes=[pk])
                if dc % 2 == 0:
                    P.op("act", lambda e: e.copy(out=xe[i][:, dc, :], in_=pg[:, 0:NS]), reads=[pk], writes=[f"f_xe{i}"])
                else:
                    P.op("dve", lambda e: e.tensor_copy(out=xe[i][:, dc, :], in_=pg[:, 0:NS]), reads=[pk], writes=[f"f_xe{i}"])
            w1v = g.w1[l][ex].rearrange("(kc p) n -> p kc n", p=128)
            w3v = g.w3[l][ex].rearrange("(kc p) n -> p kc n", p=128)
            for fp in range(4):
                wi = nw13 % 2; nw13 += 1
                P.dma("pool", lambda e: e.dma_start(out=w13[wi][:, 0, :, :], in_=w1v[:, :, fp * 256:(fp + 1) * 256]), writes=[f"f_w13_{wi}"])
                P.dma("pool", lambda e: e.dma_start(out=w13[wi][:, 1, :, :], in_=w3v[:, :, fp * 256:(fp + 1) * 256]), writes=[f"f_w13_{wi}"])
                for fh in range(2):
                    fc = fp * 2 + fh
                    pa = (fp * 2 + fh) % 2
                    for kc in range(KC):
                        P.op("pe", lambda e, kc=kc: e.matmul(psA[pa][:, 0:NS], lhsT=w13[wi][:, 0, kc, fh * 128:(fh + 1) * 128], rhs=xe[i][:, kc, :], start=(kc == 0), stop=(kc == KC - 1)),
                             reads=[f"f_w13_{wi}", f"f_xe{i}"], writes=[f"f_psA{pa}"])
                    for kc in range(KC):
                        P.op("pe", lambda e, kc=kc: e.matmul(psU[pa][:, 0:NS], lhsT=w13[wi][:, 1, kc, fh * 128:(fh + 1) * 128], rhs=xe[i][:, kc, :], start=(kc == 0), stop=(kc == KC - 1)),
                             reads=[f"f_w13_{wi}", f"f_xe{i}"], writes=[f"f_psU{pa}"])
                    P.op("act", lambda e: e.activation(out=asb[pa][:], in_=psA[pa][:, 0:NS], func=AF.Silu), reads=[f"f_psA{pa}"], writes=[f"f_a{pa}"])
                    P.op("dve", lambda e: e.tensor_tensor(out=act[:, fc, :], in0=psU[pa][:, 0:NS], in1=asb[pa][:], op=ALU.mult), reads=[f"f_psU{pa}", f"f_a{pa}"], writes=[f"f_act{fc}"])
            w2v = g.w2[l][ex].rearrange("(fc p) n -> p fc n", p=128)
            for cg in range(4):
                wi = nw2 % 2; nw2 += 1
                P.dma("pool", lambda e: e.dma_start(out=w2t[wi][:], in_=w2v[:, :, cg * 512:(cg + 1) * 512]), writes=[f"f_w2_{wi}"])
                for ch, (s0, w) in enumerate(((0, 128), (128, 128), (256, 32))):
                    py = psY[ny % 2]; pk = f"f_psY{ny % 2}"; ny += 1
                    yi = nyt % 3; nyt += 1
                    for fc in range(8):
                        P.op("pe", lambda e, fc=fc: e.matmul(py[0:w, :], lhsT=act[:, fc, s0:s0 + w], rhs=w2t[wi][:, fc, :], start=(fc == 0), stop=(fc == 7)),
                             reads=[f"f_w2_{wi}"] + [f"f_act{q}" for q in range(8)], writes=[pk])
                    P.op("dve", lambda e: e.tensor_scalar_mul(out=yt[yi][0:w, :], in0=py[0:w, :], scalar1=g.g_sl[0:w, ex, ch:ch + 1]), reads=[pk, "g_sl"], writes=[f"f_y{yi}"])
                    P.dma("sp", lambda e: e.dma_start(out=g.ybuf[ex, ch, 0:w, cg * 512:(cg + 1) * 512], in_=yt[yi][0:w, :]), reads=[f"f_y{yi}"], writes=["ybuf"])
        P.barrier()


def stage_scatter(g, l):
    nc, P = g.nc, g.P
    with contextlib.ExitStack() as es:
        sb = lambda name, shape, dt: es.enter_context(nc.sbuf_tensor(uname(name), shape, dt))
        pst = lambda name, shape, dt: es.enter_context(nc.psum_tensor(uname(name), shape, dt))
        Y = g.A[:, 0:NE * 2 * 1024].rearrange("p (e c n) -> p e c n", e=NE, c=2)
        Yc = g.A[:, 0:NE * D].rearrange("p (e n) -> p e n", e=NE)
        ti = sb("s_ti", [128, TL], I32)
        iotaF = sb("s_iotaF", [128, TL], F32)
        P.op("pool", lambda e: e.iota(ti[:, :], pattern=[[1, TL]], base=0, channel_multiplier=0), writes=["s_ti"])
        P.op("dve", lambda e: e.tensor_copy(out=iotaF[:], in_=ti[:, :]), reads=["s_ti"], writes=["iotaF"])
        Soh = sb("s_oh", [128, NE * 2, 512], BF16)
        Sc = sb("s_ohc", [128, NE, TC], BF16)
        xb = [sb(f"s_xb{i}", [128, 512], F32) for i in range(3)]
        ps = [pst(f"s_ps{i}", [128, 512], F32) for i in range(3)]
        n = 0
        for dh in range(2):
            for ex in range(NE):
                P.dma("sp", lambda e, ex=ex: e.dma_start(out=Y[:, ex, :, :], in_=g.ybuf[ex, 0:2, :, dh * 1024:(dh + 1) * 1024].rearrange("c p n -> p c n")),
                      reads=["ybuf"], writes=["A"])
            for (t0, W) in TGS[:4]:
                for ex in range(NE):
                    for ch in range(2):
                        eng = "dve"
                        P.op(eng, lambda e, ex=ex, ch=ch: e.tensor_scalar(out=Soh[:, ex * 2 + ch, :], in0=iotaF[:, t0:t0 + 512], scalar1=g.idx_sl[:, ex, ch:ch + 1], scalar2=None, op0=ALU.is_equal),
                             reads=["iotaF", "idx_sl"], writes=[f"s_oh{ex}_{ch}"])
                for dcl in range(8):
                    dc = dh * 8 + dcl
                    pi = n % 3; n += 1
                    P.dma("sp", lambda e: e.dma_start(out=xb[pi][:, 0:W], in_=g.xT[dc * 128:(dc + 1) * 128, t0:t0 + W]), reads=["xT"], writes=[f"s_xb{pi}"])
                    for ex in range(NE):
                        for ch in range(2):
                            P.op("pe", lambda e, ex=ex, ch=ch: e.matmul(ps[pi][:, 0:W], lhsT=Y[:, ex, ch, dcl * 128:(dcl + 1) * 128], rhs=Soh[:, ex * 2 + ch, :],
                                                                        start=(ex == 0 and ch == 0), stop=(ex == NE - 1 and ch == 1)),
                                 reads=["A", f"s_oh{ex}_{ch}"], writes=[f"s_ps{pi}"])
                    P.op("dve", lambda e: e.scalar_tensor_tensor(out=xb[pi][:, 0:W], in0=ps[pi][:, 0:W], scalar=g.cG[:, l, 1, dc, 0:1], in1=xb[pi][:, 0:W], op0=ALU.mult, op1=ALU.add),
                         reads=[f"s_ps{pi}", f"s_xb{pi}", "cG"], writes=[f"s_xb{pi}"])
                    P.dma("sp", lambda e: e.dma_start(out=g.xT[dc * 128:(dc + 1) * 128, t0:t0 + W], in_=xb[pi][:, 0:W]), reads=[f"s_xb{pi}"], writes=["xT"])
        P.dma("sp", lambda e: e.dma_start(out=Yc[0:CAP_C, :, :], in_=g.ybuf[:, 2, 0:CAP_C, :].rearrange("e p n -> p e n")), reads=["ybuf"], writes=["A"])
        for ex in range(NE):
            P.op("dve", lambda e, ex=ex: e.tensor_scalar(out=Sc[0:CAP_C, ex, :], in0=iotaF[0:CAP_C, 0:TC], scalar1=g.idx_sl[0:CAP_C, ex, 2:3], scalar2=None, op0=ALU.is_equal),
                 reads=["iotaF", "idx_sl"], writes=["s_ohc"])
        for dc in range(KC):
            pi = n % 3; n += 1
            P.dma("sp", lambda e: e.dma_start(out=xb[pi][:, 0:TC], in_=g.xT[dc * 128:(dc + 1) * 128, TL:T]), reads=["xT"], writes=[f"s_xb{pi}"])
            for ex in range(NE):
                P.op("pe", lambda e, ex=ex: e.matmul(ps[pi][:, 0:TC], lhsT=Yc[0:CAP_C, ex, dc * 128:(dc + 1) * 128], rhs=Sc[0:CAP_C, ex, :], start=(ex == 0), stop=(ex == NE - 1)),
                     reads=["A", "s_ohc"], writes=[f"s_ps{pi}"])
            P.op("dve", lambda e: e.scalar_tensor_tensor(out=xb[pi][:, 0:TC], in0=ps[pi][:, 0:TC], scalar=g.cG[:, l, 1, dc, 1:2], in1=xb[pi][:, 0:TC], op0=ALU.mult, op1=ALU.add),
                 reads=[f"s_ps{pi}", f"s_xb{pi}", "cG"], writes=[f"s_xb{pi}"])
            P.dma("sp", lambda e: e.dma_start(out=g.xT[dc * 128:(dc + 1) * 128, TL:T], in_=xb[pi][:, 0:TC]), reads=[f"s_xb{pi}"], writes=["xT"])
        P.barrier()


def stage_final(g):
    nc, P = g.nc, g.P
    with contextlib.ExitStack() as es:
        sb = lambda name, shape, dt: es.enter_context(nc.sbuf_tensor(uname(name), shape, dt))
        W = 256
        xb = [sb(f"n_xb{i}", [128, KC, W], F32) for i in range(2)]
        sq = [sb(f"n_sq{i}", [128, KC, W], F32) for i in range(2)]
        rstd = [sb(f"n_rstd{i}", [128, W], F32) for i in range(2)]
        ot = [sb(f"f_ot{i}", [128, D], F32) for i in range(2)]
        ps = [es.enter_context(nc.psum_tensor(uname(f"n_ps{i}"), [128, 512], F32)) for i in range(2)]
        pst = [es.enter_context(nc.psum_tensor(uname(f"f_pst{i}"), [128, 512], F32)) for i in range(4)]
        tiles = (xb, sq, rstd, None, ps)
        npt = 0
        for bi, (t0, _) in enumerate(BLKS[:TL // 256]):
            i = bi % 2
            norm_block(g, tiles, 0, 0, t0, W, i)
            for kc in range(KC):
                P.op("dve", lambda e, kc=kc: e.scalar_tensor_tensor(out=sq[i][:, kc, :], in0=xb[i][:, kc, :], scalar=g.finalg[:, kc:kc + 1],
                                                                   in1=rstd[i][:, :], op0=ALU.mult, op1=ALU.mult),
                     reads=[f"n_xb{i}", f"n_rstd{i}", "finalg", f"n_ps{i}"], writes=[f"n_sq{i}"])
            for th in range(2):
                oi = (bi * 2 + th) % 2
                for q4 in range(4):
                    pp = pst[npt % 4]
                    pk = f"f_pst{npt % 4}"
                    npt += 1
                    for q in range(4):
                        dc = q4 * 4 + q
                        P.op("pe", lambda e, pp=pp, q=q, dc=dc, th=th: e.transpose(out=pp[:, q * 128:(q + 1) * 128], in_=sq[i][:, dc, th * 128:(th + 1) * 128], identity=g.identF[:]),
                             reads=[f"n_sq{i}", "identF"], writes=[pk])
                    if q4 % 2 == 0:
                        P.op("dve", lambda e, pp=pp, q4=q4: e.tensor_copy(out=ot[oi][:, q4 * 512:(q4 + 1) * 512], in_=pp[:]), reads=[pk], writes=[f"f_ot{oi}_{q4}"])
                    else:
                        P.op("act", lambda e, pp=pp, q4=q4: e.copy(out=ot[oi][:, q4 * 512:(q4 + 1) * 512], in_=pp[:]), reads=[pk], writes=[f"f_ot{oi}_{q4}"])
                r0 = t0 + th * 128
                P.dma("sp", lambda e, r0=r0, oi=oi: e.dma_start(out=g.out[r0:r0 + 128, :], in_=ot[oi][:]),
                      reads=[f"f_ot{oi}_{q}" for q in range(4)], writes=["out"])
        P.barrier()


def host_layout(inputs, b):
    f = lambda a: np.ascontiguousarray(a, dtype=np.float32)
    c = np.asarray(inputs["c"])[b]
    cc = np.asarray(inputs["c_ctx"])
    cT = np.stack([c.reshape(KC, 128).T, cc.reshape(KC, 128).T], axis=-1)
    bada = np.asarray(inputs["b_ada"]).reshape(DEPTH, 96, 128).transpose(2, 0, 1)
    ng = np.asarray(inputs["norm_g"]).reshape(DEPTH, 2, KC, 128).transpose(3, 0, 1, 2)
    fg = np.asarray(inputs["final_g"]).reshape(KC, 128).T
    cw = np.asarray(inputs["conv_w"]).reshape(2, 3, KC, 128).transpose(3, 0, 1, 2)
    return {
        "x_in": f(np.asarray(inputs["x"])[b]), "ctx_in": f(np.asarray(inputs["ctx"])[b]),
        "cT": f(cT), "badaT": f(bada), "normgT": f(ng), "finalgT": f(fg), "convwT": f(cw),
    }


def bias_table(rpb):
    rpb = np.asarray(rpb, dtype=np.float32)
    col = np.arange(64)
    cs = np.clip(col - 8, 0, 48)
    colin = (col[None, :] >= cs[:, None]) & (col[None, :] < cs[:, None] + 16)
    dci = np.clip(col[None, :] - col[:, None] + 15, 0, 30)
    tb = rpb[:, :, :, dci]
    tb = np.where(colin[None, None, None], tb, np.float32(-1e30))
    tb = tb.transpose(0, 1, 3, 2, 4).reshape(2, NE, 64, 15 * 64)
    return np.ascontiguousarray(tb, dtype=np.float32)


def shared_inputs(inputs, names):
    out = {}
    for n in names:
        if n.startswith("biasT"):
            out[n] = bias_table(inputs["attn_rpb"])[int(n[5:])]
        elif n.startswith("w1_") or n.startswith("w3_") or n.startswith("w2_"):
            out[n] = np.ascontiguousarray(np.asarray(inputs[n[:2]])[int(n[3:])], dtype=np.float32)
        else:
            base = n.rstrip("0123456789")
            out[n] = np.ascontiguousarray(np.asarray(inputs[base])[int(n[len(base):])], dtype=np.float32)
    return out


_CACHE = {}


def kernel(**inputs):
    if "nc" not in _CACHE:
        _CACHE["nc"] = build_program()
    nc, g = _CACHE["nc"]
    shared = shared_inputs(inputs, g.names)
    in_maps = []
    for core in range(4):
        m = dict(shared)
        m.update(host_layout(inputs, core))
        in_maps.append(m)
    res = run_bass_kernel_spmd(nc, in_maps, core_ids=list(range(4)))
    out = np.stack([res.results[b]["out"] for b in range(4)], axis=0)
    return out.astype(np.float32)
```
